# Optimizing a Trainium2 kernel written in Bass

```python
import math
import jax, jax.numpy as jnp
from jax import lax
import numpy as np

D_MODEL = 1024
BATCH = 16
SEQ = 2048
DEPTH = 2

GRID_W = 64
CTX_LEN = 256
F32 = jnp.float32
EPS = 1e-6
HEAD_DIM = 64
D_MIX = D_MODEL
W_LRU = D_MIX // 4
W_RWKV = D_MIX // 4
W_MLSTM = D_MIX // 4
W_HYENA = D_MIX - W_LRU - W_RWKV - W_MLSTM
GROUP_WIDTHS = (W_LRU, W_RWKV, W_MLSTM, W_HYENA)
H_LRU = W_LRU // HEAD_DIM
H_RWKV = W_RWKV // HEAD_DIM
H_MLSTM = W_MLSTM // HEAD_DIM
CONV_W = 4
LRU_C = 8.0
RWKV_LORA_W = 32
RWKV_LORA_A = 32
RWKV_LORA_G = 64
RWKV_GN_EPS = 64e-5
MLSTM_CHUNK = 64
HYENA_ORDER = 2
HYENA_SHORT = 3
HYENA_EMB = 33
HYENA_HID = 64
HYENA_TARGET = 1e-2
HYENA_FAST = 0.3
HYENA_SLOW = 1.5
PEER_HEADS = 8
PEER_NKEYS = 128
PEER_EXPERTS = PEER_NKEYS * PEER_NKEYS
PEER_TOPK = 16
PEER_DQ = 256
PEER_BLOCK = 128
IN_SPLITS = (W_LRU, W_LRU, W_RWKV, W_RWKV, W_RWKV, RWKV_LORA_W, RWKV_LORA_A, RWKV_LORA_G, W_MLSTM, W_MLSTM, W_MLSTM, W_MLSTM, 4 * H_MLSTM, W_HYENA, W_HYENA, W_HYENA)
D_IN = sum(IN_SPLITS)

kernel_name = 'hybrid_prefix_dit_lru_rwkv_mlstm_hyena_peer'


def rmsnorm(x, g):
    xf = x.astype(F32)
    y = xf * lax.rsqrt(jnp.mean(xf * xf, axis=-1, keepdims=True) + EPS)
    return (y * g.astype(F32)).astype(x.dtype)


def modulate(h, shift, scale):
    return h * (1 + scale) + shift


def flip_if(a, d, axis=1):
    return jnp.flip(a, axis) if d == 1 else a


def split_cols(z):
    offs = np.cumsum(IN_SPLITS)[:-1].tolist()
    return jnp.split(z, offs, axis=-1)


def dwconv(x, w, b, pad_left):
    K = w.shape[0]
    y = lax.conv_general_dilated(x, w[:, None, :].astype(x.dtype), (1,), [(pad_left, K - 1 - pad_left)],
                                 dimension_numbers=('NWC', 'WIO', 'NWC'), feature_group_count=x.shape[-1])
    return y + b.astype(x.dtype)


def token_shift(x, mu):
    prev = jnp.pad(x, ((0, 0), (1, 0), (0, 0)))[:, :-1]
    nxt = jnp.pad(x, ((0, 0), (0, 1), (0, 0)))[:, 1:]
    return x + mu[0] * (prev - x) + mu[1] * (nxt - x)


def split_heads(t, h):
    Bn, L, W = t.shape
    return t.reshape(Bn, L, h, W // h)


def to_colmajor(a):
    Bn, L, C = a.shape
    rows = L // GRID_W
    return a.reshape(Bn, rows, GRID_W, C).transpose(0, 2, 1, 3).reshape(Bn, L, C)


def from_colmajor(a):
    Bn, L, C = a.shape
    rows = L // GRID_W
    return a.reshape(Bn, GRID_W, rows, C).transpose(0, 2, 1, 3).reshape(Bn, L, C)


def linear_recurrence(a, b, h0):
    def comb(l, r):
        return (l[0] * r[0], r[0] * l[1] + r[1])
    A, H = lax.associative_scan(comb, (a, b), axis=1)
    return H + A * h0[:, None, :]


def rglru_scan(xc, p, d, h0):
    Bn, L, W = xc.shape
    xh = xc.reshape(Bn, L, H_LRU, HEAD_DIM)
    r = jax.nn.sigmoid(jnp.einsum('blhi,hij->blhj', xh, p['lru_wr'][d]).reshape(Bn, L, W) + p['lru_br'][d])
    i = jax.nn.sigmoid(jnp.einsum('blhi,hij->blhj', xh, p['lru_wi'][d]).reshape(Bn, L, W) + p['lru_bi'][d])
    log_a = -LRU_C * r * jax.nn.softplus(-p['lru_lam'][d])
    b = jnp.sqrt(-jnp.expm1(2.0 * log_a)) * (i * xc)
    return linear_recurrence(jnp.exp(log_a), b, h0)


def mixer_rglru(f_c, f_l, p, need_ctx):
    (x_c, g_c), (x_l, g_l) = f_c, f_l
    xc_c = dwconv(x_c, p['lru_conv_w'], p['lru_conv_b'], CONV_W // 2).astype(F32)
    xc_l = dwconv(x_l, p['lru_conv_w'], p['lru_conv_b'], CONV_W // 2).astype(F32)
    h0 = jnp.zeros((xc_l.shape[0], W_LRU), F32)
    hs_c, hs_l = [], []
    for d in range(2):
        hc = rglru_scan(flip_if(xc_c, d), p, d, h0)
        hl = rglru_scan(flip_if(xc_l, d), p, d, hc[:, -1])
        hs_c.append(flip_if(hc, d))
        hs_l.append(flip_if(hl, d))
    y_l = jax.nn.gelu(g_l.astype(F32)) * (hs_l[0] + hs_l[1])
    y_c = jax.nn.gelu(g_c.astype(F32)) * (hs_c[0] + hs_c[1]) if need_ctx else None
    return y_c, y_l


def rwkv_shift(f, p):
    r, k, v, zw, za, zg = [t.astype(F32) for t in f]
    mu = p['rwkv_mu']
    return (token_shift(r, mu[0]), token_shift(k, mu[1]), token_shift(v, mu[2]), zw, za, zg)


def rwkv_dir_inputs(f, p, d):
    r, k, v, zw, za, _ = f
    w_log = -jax.nn.softplus(-(p['rwkv_w0'][d] + jnp.tanh(zw) @ p['rwkv_w2'][d])) - 0.5
    decay = jnp.exp(-jnp.exp(w_log))
    a = jax.nn.sigmoid(p['rwkv_a0'][d] + za @ p['rwkv_a2'][d])
    kk = split_heads(k * p['rwkv_kk'], H_RWKV)
    kk = kk / jnp.maximum(jnp.linalg.norm(kk, axis=-1, keepdims=True), 1e-12)
    kd = k * (1 + (a - 1) * p['rwkv_ka'])
    return [split_heads(r, H_RWKV), split_heads(decay, H_RWKV), split_heads(kd, H_RWKV),
            split_heads(v, H_RWKV), kk, kk * split_heads(a, H_RWKV)]


def rwkv7_scan(ins, s0):
    def step(S, inp):
        r, w, k, v, kk, kka = inp
        sa = jnp.einsum('bhvk,bhk->bhv', S, kk)
        S = S * w[:, :, None, :] - sa[..., None] * kka[:, :, None, :] + v[..., None] * k[:, :, None, :]
        return S, jnp.einsum('bhvk,bhk->bhv', S, r)
    S, ys = lax.scan(step, s0, tuple(jnp.moveaxis(t, 1, 0) for t in ins))
    return jnp.moveaxis(ys, 0, 1), S


def rwkv_bonus(ins, p):
    r, _, kd, v = ins[:4]
    return jnp.sum(r * kd * p['rwkv_rk'], axis=-1, keepdims=True) * v


def rwkv_out(y, bonus, zg, p):
    Bn, L, H, N = y.shape
    mu = jnp.mean(y, axis=-1, keepdims=True)
    var = jnp.mean(jnp.square(y - mu), axis=-1, keepdims=True)
    yn = ((y - mu) * lax.rsqrt(var + RWKV_GN_EPS)).reshape(Bn, L, H * N) * p['rwkv_ln_g'] + p['rwkv_ln_b']
    g = jax.nn.sigmoid(zg) @ p['rwkv_g2']
    return (yn + bonus.reshape(Bn, L, H * N)) * g


def mixer_rwkv(f_c, f_l, p, need_ctx):
    fc, fl = rwkv_shift(f_c, p), rwkv_shift(f_l, p)
    s0 = jnp.zeros((fl[0].shape[0], H_RWKV, HEAD_DIM, HEAD_DIM), F32)
    y_c, y_l = [], []
    for d in range(2):
        ic, il = rwkv_dir_inputs(fc, p, d), rwkv_dir_inputs(fl, p, d)
        oc, s_ctx = rwkv7_scan([flip_if(t, d) for t in ic], s0)
        ol, _ = rwkv7_scan([flip_if(t, d) for t in il], s_ctx)
        y_l.append((flip_if(ol, d), rwkv_bonus(il, p)))
        if need_ctx:
            y_c.append((flip_if(oc, d), rwkv_bonus(ic, p)))
    out_l = rwkv_out(y_l[0][0] + y_l[1][0], y_l[0][1] + y_l[1][1], fl[5], p)
    out_c = rwkv_out(y_c[0][0] + y_c[1][0], y_c[0][1] + y_c[1][1], fc[5], p) if need_ctx else None
    return out_c, out_l


def mlstm_prep(f, p):
    q, k, v, o, gz = f
    Bn, L, _ = q.shape
    qk = jax.nn.silu(dwconv(jnp.concatenate([q, k], axis=-1), p['mlstm_conv_w'], p['mlstm_conv_b'], CONV_W // 2)).astype(F32)
    q, k = jnp.split(qk, 2, axis=-1)
    heads = lambda t: t.reshape(Bn, L, H_MLSTM, HEAD_DIM).transpose(0, 2, 1, 3)
    gates = (gz.astype(F32).reshape(Bn, L, 2, 2, H_MLSTM) + p['mlstm_gate_b']).transpose(2, 3, 0, 4, 1)
    return heads(q) * HEAD_DIM ** -0.5, heads(k), heads(v.astype(F32)), o.astype(F32), gates


def mlstm_chunkwise(q, k, v, ig, lf, state):
    Bn, H, L, N = q.shape
    T = MLSTM_CHUNK
    nc = L // T
    ch = lambda a: jnp.moveaxis(a.reshape((Bn, H, nc, T) + a.shape[3:]), 2, 0)
    mask = jnp.tril(jnp.ones((T, T), dtype=bool))

    def step(carry, inp):
        C, n, m = carry
        qc, kc, vc, ic, fc = inp
        b = jnp.cumsum(fc, axis=-1)
        logd = jnp.where(mask, b[..., :, None] - b[..., None, :] + ic[..., None, :], -jnp.inf)
        inter = b + m[..., None]
        mt = jnp.maximum(inter, jnp.max(logd, axis=-1))
        s = jnp.einsum('bhtn,bhsn->bhts', qc, kc) * jnp.exp(logd - mt[..., None])
        e_inter = jnp.exp(inter - mt)
        num = jnp.einsum('bhts,bhsn->bhtn', s, vc) + e_inter[..., None] * jnp.einsum('bhvk,bhtk->bhtv', C, qc)
        den = jnp.sum(s, axis=-1) + e_inter * jnp.einsum('bhk,bhtk->bht', n, qc)
        h = num / jnp.maximum(jnp.abs(den), jnp.exp(-mt))[..., None]
        bT = b[..., -1]
        logw = bT[..., None] - b + ic
        m_new = jnp.maximum(bT + m, jnp.max(logw, axis=-1))
        wgt = jnp.exp(logw - m_new[..., None])
        dec = jnp.exp(bT + m - m_new)
        C = dec[..., None, None] * C + jnp.einsum('bhs,bhsv,bhsk->bhvk', wgt, vc, kc)
        n = dec[..., None] * n + jnp.einsum('bhs,bhsk->bhk', wgt, kc)
        return (C, n, m_new), h

    state, hs = lax.scan(step, state, (ch(q), ch(k), ch(v), ch(ig), ch(lf)))
    return jnp.moveaxis(hs, 0, 2).reshape(Bn, H, L, N), state


def mixer_mlstm(f_c, f_l, p, need_ctx):
    f_l = [to_colmajor(t) for t in f_l]
    qc, kc, vc, oc, gc = mlstm_prep(f_c, p)
    ql, kl, vl, ol, gl = mlstm_prep(f_l, p)
    Bn = ql.shape[0]
    st0 = (jnp.zeros((Bn, H_MLSTM, HEAD_DIM, HEAD_DIM), F32), jnp.zeros((Bn, H_MLSTM, HEAD_DIM), F32),
           jnp.zeros((Bn, H_MLSTM), F32))
    h_c, h_l = [], []
    for d in range(2):
        hc, st = mlstm_chunkwise(flip_if(qc, d, 2), flip_if(kc, d, 2), flip_if(vc, d, 2), flip_if(gc[d, 0], d, 2),
                                 jax.nn.log_sigmoid(flip_if(gc[d, 1], d, 2)), st0)
        hl, _ = mlstm_chunkwise(flip_if(ql, d, 2), flip_if(kl, d, 2), flip_if(vl, d, 2), flip_if(gl[d, 0], d, 2),
                                jax.nn.log_sigmoid(flip_if(gl[d, 1], d, 2)), st)
        h_c.append(flip_if(hc, d, 2))
        h_l.append(flip_if(hl, d, 2))
    merge_heads = lambda h: h.transpose(0, 2, 1, 3).reshape(h.shape[0], h.shape[2], W_MLSTM)
    y_l = from_colmajor(jax.nn.sigmoid(ol) * merge_heads(h_l[0] + h_l[1]))
    y_c = jax.nn.sigmoid(oc) * merge_heads(h_c[0] + h_c[1]) if need_ctx else None
    return y_c, y_l


def hyena_spectrum(L, p):
    pos = jnp.arange(L, dtype=F32)
    t = pos / (L - 1)
    bands = (HYENA_EMB - 1) // 2
    freqs = jnp.linspace(1e-4, bands - 1, bands, dtype=F32)
    ang = (2 * math.pi / L) * pos[:, None] * freqs[None, :]
    z = jnp.concatenate([t[:, None], jnp.cos(ang), -jnp.sin(ang)], axis=-1)
    h = jnp.sin(p['hy_freq'][0] * (z @ p['hy_w1'] + p['hy_b1']))
    h = jnp.sin(p['hy_freq'][1] * (h @ p['hy_w2'] + p['hy_b2']))
    h = (h @ p['hy_w3']).astype(F32).reshape(L, HYENA_ORDER, 2, W_HYENA)
    deltas = jnp.abs(jnp.linspace(math.log(HYENA_TARGET) / HYENA_SLOW, math.log(HYENA_TARGET) / HYENA_FAST,
                                  W_HYENA, dtype=F32))
    h = h * jnp.exp(-t[:, None, None, None] * deltas)
    fwd, bwd = h[:, :, 0], h[:, :, 1]
    two = jnp.concatenate([fwd, jnp.zeros_like(fwd[:1]), jnp.flip(bwd[1:], axis=0)], axis=0)
    two = two / (jnp.sum(jnp.abs(two), axis=0, keepdims=True) + EPS)
    return jnp.fft.rfft(two, axis=0)


def long_conv(u, spec, bias):
    L = u.shape[1]
    y = jnp.fft.irfft(jnp.fft.rfft(u, n=2 * L, axis=1) * spec, n=2 * L, axis=1)[:, :L]
    return y + u * bias


def mixer_hyena(f, p):
    u = dwconv(jnp.concatenate(f, axis=-1), p['hy_conv_w'], p['hy_conv_b'], HYENA_SHORT // 2).astype(F32)
    v, x1, x2 = jnp.split(u, 3, axis=-1)
    spec = hyena_spectrum(u.shape[1], p)
    z = x1 * long_conv(v, spec[:, 0], p['hy_bias'][0])
    return x2 * long_conv(z, spec[:, 1], p['hy_bias'][1])


def peer_ffn(h, p):
    Bn, L, D = h.shape
    wq, keys, u_tab, v_tab = p['peer_wq'], p['peer_keys'].astype(F32), p['peer_u'], p['peer_v']

    def token_block(xb):
        T = xb.shape[0]
        q = (xb @ wq).astype(F32).reshape(T, PEER_HEADS, 2, PEER_DQ // 2)
        s = jnp.einsum('thcq,hckq->thck', q, keys)
        sv, si = lax.top_k(s, PEER_TOPK)
        cand = (sv[:, :, 0, :, None] + sv[:, :, 1, None, :]).reshape(T, PEER_HEADS, PEER_TOPK * PEER_TOPK)
        cidx = (si[:, :, 0, :, None] * PEER_NKEYS + si[:, :, 1, None, :]).reshape(T, PEER_HEADS, PEER_TOPK * PEER_TOPK)
        top_s, top_j = lax.top_k(cand, PEER_TOPK)
        eidx = jnp.take_along_axis(cidx, top_j, axis=-1)
        gate = jax.nn.softmax(top_s, axis=-1)
        act = jax.nn.gelu(jnp.einsum('td,thkd->thk', xb, u_tab[eidx]).astype(F32))
        return jnp.einsum('thk,thkd->td', (gate * act).astype(xb.dtype), v_tab[eidx])

    out = lax.map(token_block, h.reshape(-1, PEER_BLOCK, D))
    return out.reshape(Bn, L, D).astype(h.dtype)


def merge_groups(ys, g, dtype):
    outs, off = [], 0
    for y, w in zip(ys, GROUP_WIDTHS):
        outs.append(rmsnorm(y, g[off:off + w]).astype(dtype))
        off += w
    return jnp.concatenate(outs, axis=-1)


def token_mixers(h_c, h_l, p, need_ctx):
    zc = split_cols(h_c @ p['w_in'])
    zl = split_cols(h_l @ p['w_in'])
    a_c, a_l = mixer_rglru(zc[0:2], zl[0:2], p, need_ctx)
    b_c, b_l = mixer_rwkv(zc[2:8], zl[2:8], p, need_ctx)
    m_c, m_l = mixer_mlstm(zc[8:13], zl[8:13], p, need_ctx)
    d_l = mixer_hyena(zl[13:16], p)
    o_l = merge_groups([a_l, b_l, m_l, d_l], p['grp_g'], h_l.dtype) @ p['w_out']
    if not need_ctx:
        return None, o_l
    d_c = mixer_hyena(zc[13:16], p)
    o_c = merge_groups([a_c, b_c, m_c, d_c], p['grp_g'], h_c.dtype) @ p['w_out']
    return o_c, o_l


def trunk_layer(x_l, x_c, c, c_ctx, p, need_ctx):
    Bn = c.shape[0]
    mod_l = (jax.nn.silu(c) @ p['ada_w'] + p['ada_b']).reshape(Bn, 6, 1, D_MODEL)
    mod_c = (jax.nn.silu(c_ctx) @ p['ada_w'] + p['ada_b']).reshape(6, 1, 1, D_MODEL)
    h_l = modulate(rmsnorm(x_l, p['norm1_g']), mod_l[:, 0], mod_l[:, 1])
    h_c = modulate(rmsnorm(x_c, p['norm1_g']), mod_c[0], mod_c[1])
    o_c, o_l = token_mixers(h_c, h_l, p, need_ctx)
    x_l = x_l + mod_l[:, 2] * o_l
    x_l = x_l + mod_l[:, 5] * peer_ffn(modulate(rmsnorm(x_l, p['norm2_g']), mod_l[:, 3], mod_l[:, 4]), p)
    if need_ctx:
        x_c = x_c + mod_c[2] * o_c
        x_c = x_c + mod_c[5] * peer_ffn(modulate(rmsnorm(x_c, p['norm2_g']), mod_c[3], mod_c[4]), p)
    return x_l, x_c


def setup_inputs(seed: int = 0) -> dict:
    key = jax.random.key(seed)
    ks = iter(jax.random.split(key, 64))

    def nrm(shape, s):
        return s * jax.random.normal(next(ks), shape, F32)

    def gain(shape):
        return 1.0 + nrm(shape, 0.02)

    L_ = DEPTH
    u_lam = jax.random.uniform(next(ks), (L_, 2, W_LRU), F32, 0.9, 0.999)
    s_lam = u_lam ** (1.0 / LRU_C)
    w0 = jnp.linspace(-6.0, -1.0, W_RWKV, dtype=F32) + nrm((L_, 2, W_RWKV), 0.1)
    gate_b = jnp.stack([nrm((L_, 2, H_MLSTM), 0.1),
                        jnp.linspace(3.0, 6.0, H_MLSTM, dtype=F32) + nrm((L_, 2, H_MLSTM), 0.1)], axis=2)
    return dict(
        x=nrm((BATCH, SEQ, D_MODEL), 1.0),
        c=nrm((BATCH, D_MODEL), 1.0),
        ctx=nrm((BATCH, CTX_LEN, D_MODEL), 1.0),
        c_ctx=nrm((D_MODEL,), 1.0),
        ada_w=nrm((L_, D_MODEL, 6 * D_MODEL), 0.5 * D_MODEL ** -0.5),
        ada_b=nrm((L_, 6 * D_MODEL), 0.01),
        norm1_g=gain((L_, D_MODEL)),
        norm2_g=gain((L_, D_MODEL)),
        w_in=nrm((L_, D_MODEL, D_IN), D_MODEL ** -0.5),
        w_out=nrm((L_, D_MIX, D_MODEL), D_MIX ** -0.5),
        grp_g=gain((L_, D_MIX)),
        lru_conv_w=nrm((L_, CONV_W, W_LRU), CONV_W ** -0.5),
        lru_conv_b=nrm((L_, W_LRU), 0.01),
        lru_wr=nrm((L_, 2, H_LRU, HEAD_DIM, HEAD_DIM), HEAD_DIM ** -0.5),
        lru_br=nrm((L_, 2, W_LRU), 0.01),
        lru_wi=nrm((L_, 2, H_LRU, HEAD_DIM, HEAD_DIM), HEAD_DIM ** -0.5),
        lru_bi=nrm((L_, 2, W_LRU), 0.01),
        lru_lam=jnp.log(s_lam) - jnp.log1p(-s_lam),
        rwkv_mu=jax.random.uniform(next(ks), (L_, 3, 2, W_RWKV), F32, 0.0, 0.4),
        rwkv_w0=w0,
        rwkv_w2=nrm((L_, 2, RWKV_LORA_W, W_RWKV), 0.5 * RWKV_LORA_W ** -0.5),
        rwkv_a0=nrm((L_, 2, W_RWKV), 0.1),
        rwkv_a2=nrm((L_, 2, RWKV_LORA_A, W_RWKV), 0.5 * RWKV_LORA_A ** -0.5),
        rwkv_g2=nrm((L_, RWKV_LORA_G, W_RWKV), RWKV_LORA_G ** -0.5),
        rwkv_kk=1.0 + nrm((L_, W_RWKV), 0.1),
        rwkv_ka=1.0 + nrm((L_, W_RWKV), 0.1),
        rwkv_rk=nrm((L_, H_RWKV, HEAD_DIM), 0.1),
        rwkv_ln_g=gain((L_, W_RWKV)),
        rwkv_ln_b=nrm((L_, W_RWKV), 0.01),
        mlstm_conv_w=nrm((L_, CONV_W, 2 * W_MLSTM), CONV_W ** -0.5),
        mlstm_conv_b=nrm((L_, 2 * W_MLSTM), 0.01),
        mlstm_gate_b=gate_b,
        hy_conv_w=nrm((L_, HYENA_SHORT, 3 * W_HYENA), HYENA_SHORT ** -0.5),
        hy_conv_b=nrm((L_, 3 * W_HYENA), 0.01),
        hy_w1=nrm((L_, HYENA_EMB, HYENA_HID), HYENA_EMB ** -0.5),
        hy_b1=nrm((L_, HYENA_HID), 0.01),
        hy_w2=nrm((L_, HYENA_HID, HYENA_HID), HYENA_HID ** -0.5),
        hy_b2=nrm((L_, HYENA_HID), 0.01),
        hy_w3=nrm((L_, HYENA_HID, HYENA_ORDER * 2 * W_HYENA), HYENA_HID ** -0.5),
        hy_freq=1.0 + nrm((L_, 2, HYENA_HID), 0.1),
        hy_bias=nrm((L_, HYENA_ORDER, W_HYENA), 0.1),
        peer_wq=nrm((L_, D_MODEL, PEER_HEADS * PEER_DQ), D_MODEL ** -0.5),
        peer_keys=nrm((L_, PEER_HEADS, 2, PEER_NKEYS, PEER_DQ // 2), (PEER_DQ // 2) ** -0.5),
        peer_u=nrm((L_, PEER_EXPERTS, D_MODEL), D_MODEL ** -0.5),
        peer_v=nrm((L_, PEER_EXPERTS, D_MODEL), PEER_HEADS ** -0.5),
        final_g=gain((D_MODEL,)),
    )


def reference(x, c, ctx, c_ctx, ada_w, ada_b, norm1_g, norm2_g, w_in, w_out, grp_g,
              lru_conv_w, lru_conv_b, lru_wr, lru_br, lru_wi, lru_bi, lru_lam,
              rwkv_mu, rwkv_w0, rwkv_w2, rwkv_a0, rwkv_a2, rwkv_g2, rwkv_kk, rwkv_ka, rwkv_rk, rwkv_ln_g, rwkv_ln_b,
              mlstm_conv_w, mlstm_conv_b, mlstm_gate_b,
              hy_conv_w, hy_conv_b, hy_w1, hy_b1, hy_w2, hy_b2, hy_w3, hy_freq, hy_bias,
              peer_wq, peer_keys, peer_u, peer_v, final_g):
    x_l, x_c = x, ctx
    for i in range(DEPTH):
        p = dict(ada_w=ada_w[i], ada_b=ada_b[i], norm1_g=norm1_g[i], norm2_g=norm2_g[i], w_in=w_in[i],
                 w_out=w_out[i], grp_g=grp_g[i],
                 lru_conv_w=lru_conv_w[i], lru_conv_b=lru_conv_b[i], lru_wr=lru_wr[i], lru_br=lru_br[i],
                 lru_wi=lru_wi[i], lru_bi=lru_bi[i], lru_lam=lru_lam[i],
                 rwkv_mu=rwkv_mu[i], rwkv_w0=rwkv_w0[i], rwkv_w2=rwkv_w2[i], rwkv_a0=rwkv_a0[i],
                 rwkv_a2=rwkv_a2[i], rwkv_g2=rwkv_g2[i], rwkv_kk=rwkv_kk[i], rwkv_ka=rwkv_ka[i],
                 rwkv_rk=rwkv_rk[i], rwkv_ln_g=rwkv_ln_g[i], rwkv_ln_b=rwkv_ln_b[i],
                 mlstm_conv_w=mlstm_conv_w[i], mlstm_conv_b=mlstm_conv_b[i], mlstm_gate_b=mlstm_gate_b[i],
                 hy_conv_w=hy_conv_w[i], hy_conv_b=hy_conv_b[i], hy_w1=hy_w1[i], hy_b1=hy_b1[i],
                 hy_w2=hy_w2[i], hy_b2=hy_b2[i], hy_w3=hy_w3[i], hy_freq=hy_freq[i], hy_bias=hy_bias[i],
                 peer_wq=peer_wq[i], peer_keys=peer_keys[i], peer_u=peer_u[i], peer_v=peer_v[i])
        x_l, x_c = trunk_layer(x_l, x_c, c, c_ctx, p, i < DEPTH - 1)
    return rmsnorm(x_l, final_g)
```

```python
from concourse.bass_utils import run_bass_kernel_spmd
import numpy as np
import concourse.bass as bass
import concourse.mybir as mybir

F32 = mybir.dt.float32
BF16 = mybir.dt.bfloat16
I32 = mybir.dt.int32
U32 = mybir.dt.uint32
AF = mybir.ActivationFunctionType
ALU = mybir.AluOpType
AX = mybir.AxisListType
NDS = 12
EPS = 1e-6


class Cfg:
    def __init__(s, D=1024, L=2048, LC=256, GW=64, NB=2, PH=8, NK=128, depth=2):
        s.D, s.L, s.LC, s.GW, s.NB, s.PH, s.NK, s.depth = D, L, LC, GW, NB, PH, NK, depth
        s.KD = D // 128
        s.LT = L + LC
        s.W = D // 4
        s.H = s.W // 64
        W, H = s.W, s.H
        s.splits = (W, W, W, W, W, 32, 32, 64, W, W, W, W, 4 * H, W, W, W)
        s.DIN = sum(s.splits)
        s.off = np.concatenate([[0], np.cumsum(s.splits)]).tolist()
        s.DQ = 2 * 128
        s.NE = NK * NK


class KB:
    def __init__(self, nc):
        self.nc = nc
        self.eng = {"pe": nc.tensor, "dve": nc.vector, "act": nc.scalar, "pool": nc.gpsimd, "sp": nc.sync}
        self.sem, self.cnt = {}, {}
        for e in self.eng:
            self.sem[e] = nc.semaphore("s_" + e).__enter__()
            self.cnt[e] = 0
        self.dq = {}
        for q in ("sp", "act", "pool"):
            self.dq[q] = [q, [nc.semaphore("d_%s%d" % (q, i)).__enter__() for i in range(NDS)], 0]
        self.lastw, self.readers, self.waited = {}, {}, {}
        self.n_instr = 0

    def _wait(self, e, tk, val):
        if tk[0] == "e":
            key = (e,) + tk
            if self.waited.get(key, 0) >= val:
                return
            self.waited[key] = val
            self.eng[e].wait_ge(self.sem[tk[1]], val)
        else:
            v = 16 * (val // NDS + 1)
            key = (e,) + tk
            if self.waited.get(key, 0) >= v:
                return
            self.waited[key] = v
            self.eng[e].wait_ge(self.dq[tk[1]][1][tk[2]], v)
        self.n_instr += 1

    def _deps(self, e, reads, writes):
        need = {}
        for r in reads:
            lw = self.lastw.get(r)
            if lw is not None:
                need[lw[0]] = max(need.get(lw[0], -1), lw[1])
        for w in writes:
            lw = self.lastw.get(w)
            if lw is not None:
                need[lw[0]] = max(need.get(lw[0], -1), lw[1])
            for tk, v in self.readers.get(w, {}).items():
                need[tk] = max(need.get(tk, -1), v)
        for tk, v in need.items():
            self._wait(e, tk, v)

    def _commit(self, tk, val, reads, writes):
        for r in reads:
            d = self.readers.setdefault(r, {})
            d[tk] = max(d.get(tk, -1), val)
        for w in writes:
            self.lastw[w] = (tk, val)
            self.readers[w] = {}

    def op(self, e, fn, reads=(), writes=()):
        self._deps(e, reads, writes)
        ins = fn(self.eng[e])
        self.cnt[e] += 1
        ins.then_inc(self.sem[e], 1)
        self._commit(("e", e), self.cnt[e], reads, writes)
        self.n_instr += 1
        return ins

    def dma(self, q, out, in_, reads=(), writes=(), **kw):
        e, sems, i = self.dq[q]
        self._deps(e, reads, writes)
        if i >= NDS:
            self._wait(e, ("d", q, i % NDS), i - NDS)
        ins = self.eng[e].dma_start(out=out, in_=in_, **kw)
        ins.then_inc(sems[i % NDS], 16)
        self.dq[q][2] = i + 1
        self._commit(("d", q, i % NDS), i, reads, writes)
        self.n_instr += 1
        return ins

    def finish(self):
        for f in self.eng:
            if self.cnt[f] > 0:
                self._wait("sp", ("e", f), self.cnt[f])
        for q in self.dq:
            n = self.dq[q][2]
            for i in range(max(0, n - NDS), n):
                self._wait("sp", ("d", q, i % NDS), i)


def key(ap):
    return ap.tensor.name


class B:
    def __init__(self, nc, cfg):
        self.nc, self.cfg = nc, cfg
        self.k = KB(nc)
        self.dmaq = 0
        self.scopes = [[]]

    def sb(self, name, shape, dt=F32):
        self.uid = getattr(self, "uid", 0) + 1
        name = "%s_u%d" % (name, self.uid)
        gd = self.nc.sbuf_tensor(name, list(shape), dt)
        t = gd.__enter__()
        self.scopes[-1].append(gd)
        return t.ap() if hasattr(t, "ap") and callable(t.ap) else t

    def push(self):
        self.scopes.append([])

    def pop(self):
        self.barrier()
        for gd in reversed(self.scopes.pop()):
            gd.__exit__(None, None, None)

    def barrier(self):
        k = self.k
        for e in k.eng:
            for f in k.eng:
                if f != e and k.cnt[f] > 0:
                    k._wait(e, ("e", f), k.cnt[f])
            for q in k.dq:
                n = k.dq[q][2]
                for i in range(max(0, n - NDS), n):
                    k._wait(e, ("d", q, i % NDS), i)

    def ps(self, name, shape, dt=F32):
        return self.nc.alloc_psum_tensor(name, list(shape), dt).ap()

    def dram(self, name, shape, dt=F32, kind="Internal"):
        return self.nc.dram_tensor(name, list(shape), dt, kind=kind).ap()

    def _rw(self, outs, ins):
        return [key(a) for a in ins if hasattr(a, "tensor")], [key(a) for a in outs]

    def dma(self, out, in_, q=None, **kw):
        if q is None:
            q = "sp"
        r, w = self._rw([out], [in_])
        return self.k.dma(q, out, in_, reads=r, writes=w, **kw)

    def act(self, out, in_, func, bias=0.0, scale=1.0, accum_out=None, e="act"):
        r, w = self._rw([out] + ([accum_out] if accum_out is not None else []), [in_, bias, scale])
        kw = {}
        if accum_out is not None:
            kw["accum_out"] = accum_out
        return self.k.op(e, lambda E: E.activation(out=out, in_=in_, func=func, bias=bias, scale=scale, **kw), r, w)

    def tt(self, out, in0, in1, op, e="dve"):
        r, w = self._rw([out], [in0, in1])
        return self.k.op(e, lambda E: E.tensor_tensor(out=out, in0=in0, in1=in1, op=op), r, w)

    def ts(self, out, in0, s1, s2=None, op0=ALU.mult, op1=None, e="dve", accum_out=None):
        r, w = self._rw([out] + ([accum_out] if accum_out is not None else []), [in0, s1, s2])
        kw = {}
        if op1 is not None:
            kw["op1"] = op1
        if accum_out is not None:
            kw["accum_out"] = accum_out
        return self.k.op(e, lambda E: E.tensor_scalar(out=out, in0=in0, scalar1=s1, scalar2=s2, op0=op0, **kw), r, w)

    def stt(self, out, in0, scalar, in1, op0, op1, e="dve"):
        r, w = self._rw([out], [in0, scalar, in1])
        return self.k.op(e, lambda E: E.scalar_tensor_tensor(out=out, in0=in0, scalar=scalar, in1=in1, op0=op0, op1=op1), r, w)

    def copy(self, out, in_, e="dve"):
        r, w = self._rw([out], [in_])
        if e == "act":
            return self.k.op(e, lambda E: E.copy(out=out, in_=in_), r, w)
        return self.k.op(e, lambda E: E.tensor_copy(out=out, in_=in_), r, w)

    def memset(self, out, val, e="dve"):
        r, w = self._rw([out], [])
        return self.k.op(e, lambda E: E.memset(out, val), r, w)

    def reduce(self, out, in_, op=ALU.add, axis=AX.X, e="dve"):
        r, w = self._rw([out], [in_])
        return self.k.op(e, lambda E: E.tensor_reduce(out=out, in_=in_, axis=axis, op=op), r, w)

    def scan(self, out, d0, d1, init, op0=ALU.mult, op1=ALU.add, e="dve"):
        r, w = self._rw([out], [d0, d1, init])
        return self.k.op(e, lambda E: E.tensor_tensor_scan(out=out, data0=d0, data1=d1, initial=init, op0=op0, op1=op1), r, w)

    def recip(self, out, in_):
        r, w = self._rw([out], [in_])
        return self.k.op("dve", lambda E: E.reciprocal(out=out, in_=in_), r, w)

    def mm(self, out, lhsT, rhs, start=True, stop=True):
        r, w = self._rw([out], [lhsT, rhs])
        if not start:
            r = r + w
        return self.k.op("pe", lambda E: E.matmul(out, lhsT, rhs, start=start, stop=stop), r, w)

    def tr(self, out, in_, ident):
        r, w = self._rw([out], [in_, ident])
        return self.k.op("pe", lambda E: E.transpose(out=out, in_=in_, identity=ident), r, w)


def mkap(t, offset, pairs):
    return bass.AP(t.tensor, offset, [list(p) for p in pairs])


def fview(t, pairs, off=0):
    return bass.AP(t.tensor, t.offset + off, [list(t.ap[0])] + [list(p) for p in pairs])


def rev(t):
    n = t.shape[-1]
    st = t.ap[-1][0]
    return bass.AP(t.tensor, t.offset + (n - 1) * st, [list(p) for p in t.ap[:-1]] + [[-st, n]])


WNAMES = ["ada_w", "ada_b", "norm1_g", "norm2_g", "w_in", "w_out", "grp_g",
          "lru_conv_w", "lru_conv_b", "lru_wr", "lru_br", "lru_wi", "lru_bi", "lru_lam",
          "rwkv_mu", "rwkv_w0", "rwkv_w2", "rwkv_a0", "rwkv_a2", "rwkv_g2", "rwkv_kk", "rwkv_ka", "rwkv_rk",
          "rwkv_ln_g", "rwkv_ln_b", "mlstm_conv_w", "mlstm_conv_b", "mlstm_gate_b",
          "hy_conv_w", "hy_conv_b", "hy_w1", "hy_b1", "hy_w2", "hy_b2", "hy_w3", "hy_freq", "hy_bias",
          "peer_wq", "peer_v", "final_g"]


def host_layout(inp):
    return {"peer_keysT": np.ascontiguousarray(np.transpose(inp["peer_keys"], (0, 1, 2, 4, 3))),
            "peer_uT": np.ascontiguousarray(np.transpose(inp["peer_u"], (0, 2, 1)))}


def wshapes(c):
    Dp, W, H = c.depth, c.W, c.H
    return dict(
        ada_w=(Dp, c.D, 6 * c.D), ada_b=(Dp, 6 * c.D), norm1_g=(Dp, c.D), norm2_g=(Dp, c.D), w_in=(Dp, c.D, c.DIN),
        w_out=(Dp, c.D, c.D), grp_g=(Dp, c.D), lru_conv_w=(Dp, 4, W), lru_conv_b=(Dp, W), lru_wr=(Dp, 2, H, 64, 64),
        lru_br=(Dp, 2, W), lru_wi=(Dp, 2, H, 64, 64), lru_bi=(Dp, 2, W), lru_lam=(Dp, 2, W),
        rwkv_mu=(Dp, 3, 2, W), rwkv_w0=(Dp, 2, W), rwkv_w2=(Dp, 2, 32, W), rwkv_a0=(Dp, 2, W), rwkv_a2=(Dp, 2, 32, W),
        rwkv_g2=(Dp, 64, W), rwkv_kk=(Dp, W), rwkv_ka=(Dp, W), rwkv_rk=(Dp, H, 64), rwkv_ln_g=(Dp, W), rwkv_ln_b=(Dp, W),
        mlstm_conv_w=(Dp, 4, 2 * W), mlstm_conv_b=(Dp, 2 * W), mlstm_gate_b=(Dp, 2, 2, H),
        hy_conv_w=(Dp, 3, 3 * W), hy_conv_b=(Dp, 3 * W), hy_w1=(Dp, 33, 64), hy_b1=(Dp, 64), hy_w2=(Dp, 64, 64),
        hy_b2=(Dp, 64), hy_w3=(Dp, 64, 4 * W), hy_freq=(Dp, 2, 64), hy_bias=(Dp, 2, W),
        peer_wq=(Dp, c.D, c.PH * c.DQ), peer_v=(Dp, c.NE, c.D),
        final_g=(c.D,))


class Ctx:
    pass


def setup(cfg, stop_after=None):
    nc = bass.Bass("TRN2", target_bir_lowering=False)
    b = B(nc, cfg)
    c = cfg
    g = Ctx()
    g.b, g.c, g.nc = b, c, nc
    g.x = b.dram("x", [c.NB, c.L, c.D], kind="ExternalInput")
    g.ctx = b.dram("ctx", [c.NB, c.LC, c.D], kind="ExternalInput")
    g.c3T = b.dram("c3T", [128, c.KD, 3], kind="ExternalInput")
    g.w = {}
    for n, s in wshapes(c).items():
        g.w[n] = b.dram(n, list(s), kind="ExternalInput")
    g.w["peer_keysT"] = b.dram("peer_keysT", [c.depth, c.PH, 2, 128, c.NK], kind="ExternalInput")
    g.w["peer_uT"] = b.dram("peer_uT", [c.depth, c.D, c.NE], kind="ExternalInput")
    g.out = b.dram("out", [c.NB, c.L, c.D], kind="ExternalOutput")
    g.modv = b.dram("modv", [3, 6, c.D])
    g.zT = b.dram("zT", [c.NB, c.DIN, c.LT])
    g.zt = b.dram("zt", [c.NB, c.LT, c.DIN])
    g.xres = b.dram("xres", [c.NB, c.LT, c.D])
    g.ymix = b.dram("ymix", [c.NB, c.LT, c.D])
    g.mlsc = b.dram("mlsc", [2, c.H, c.LT])
    g.mlsc2 = b.dram("mlsc2", [2, 2, c.H, c.LT])
    g.ident = b.sb("ident", [128, 128])
    g.identb = b.sb("identb", [128, 128], BF16)
    io = b.sb("iota_t", [128, 128])
    b.k.op("pool", lambda E: E.iota(io, pattern=[[1, 128]], base=0, channel_multiplier=-1, allow_small_or_imprecise_dtypes=True), [], [key(io)])
    b.ts(g.ident, io, 0.0, None, op0=ALU.is_equal)
    b.copy(g.identb, g.ident)
    g.negpi = b.sb("negpi", [128, 1])
    b.memset(g.negpi, -float(np.pi))
    hy_decl(g)
    rw_decl(g)
    g.neghalf = b.sb("neghalf", [128, 1])
    b.memset(g.neghalf, -0.5)
    g.pb = [b.ps("pb%d" % i, [128, 512]) for i in range(6)]
    g.pbb = [b.ps("pbb%d" % i, [128, 1024], BF16) for i in range(2)]
    return g


def stage_mod(g, l):
    b, c = g.b, g.c
    D, KD = c.D, c.KD
    c3 = b.sb("c3_%d" % l, [128, KD, 3])
    b.dma(c3, g.c3T)
    b.act(c3, c3, AF.Silu)
    modrow = b.sb("modrow%d" % l, [3, 6 * D])
    adab = b.sb("adab%d" % l, [3, 6 * D])
    b.dma(adab, g.w["ada_b"][l:l + 1, :].partition_broadcast(3) if False else mkap(g.w["ada_b"], l * 6 * D, [[0, 3], [1, 6 * D]]))
    aw = g.w["ada_w"][l].rearrange("(kd p) c -> p kd c", p=128)
    wts = [b.sb("adaw%d_%d" % (l, i), [128, KD, 512]) for i in range(2)]
    for cc in range(6 * D // 512):
        wt = wts[cc % 2]
        b.dma(wt, aw[:, :, cc * 512:(cc + 1) * 512])
        pp = g.pb[cc % 2]
        for kd in range(KD):
            b.mm(pp[0:3, :], c3[:, kd, :], wt[:, kd, :], start=(kd == 0), stop=(kd == KD - 1))
        b.tt(modrow[:, cc * 512:(cc + 1) * 512], pp[0:3, :], adab[:, cc * 512:(cc + 1) * 512], ALU.add)
    for slot, gn in ((1, "norm1_g"), (4, "norm2_g")):
        gr = b.sb("gr%d_%d" % (l, slot), [3, D])
        b.dma(gr, mkap(g.w[gn], l * D, [[0, 3], [1, D]]))
        sl = modrow[:, slot * D:(slot + 1) * D]
        b.stt(sl, sl, 1.0, gr, ALU.add, ALU.mult)
    b.dma(g.modv.rearrange("r s d -> r (s d)"), modrow)


def bcast_row(g, name, src_ap_dram, off, n, parts=128, t=None):
    if t is None:
        t = g.b.sb(name, [parts, n])
    g.b.dma(t, mkap(src_ap_dram, off, [[0, parts], [1, n]]))
    return t


def stage_in(g, l):
    b, c = g.b, g.c
    D, KD, DIN = c.D, c.KD, c.DIN
    wbf = b.sb("wbf", [128, KD, DIN], BF16)
    wst = [b.sb("wst%d" % i, [128, DIN]) for i in range(2)]
    for kd in range(KD):
        b.dma(wst[kd % 2], g.w["w_in"][l, kd * 128:(kd + 1) * 128, :])
        b.copy(wbf[:, kd, :], wst[kd % 2], e=("dve" if kd % 2 else "pool"))
    hT = b.sb("hT", [128, KD, c.L], BF16)
    xt = [b.sb("xt%d" % i, [128, D]) for i in range(2)]
    hb = [b.sb("hb%d" % i, [128, D], BF16) for i in range(2)]
    junk = b.sb("junk", [128, D])
    ss = b.sb("ss", [128, 1])
    stg = [b.sb("stg%d" % i, [128, 512]) for i in range(2)]
    stg2 = [b.sb("stg2_%d" % i, [128, DIN]) for i in range(2)]
    nst = 0
    a1 = b.sb("a1row", [128, D])
    s1 = b.sb("s1row", [128, D])
    for bi in range(c.NB):
        for seg in range(2):
            r = 2 if seg == 0 else bi
            Lu = c.LC if seg == 0 else c.L
            col0 = 0 if seg == 0 else c.LC
            if l == 0:
                src = g.ctx[bi] if seg == 0 else g.x[bi]
            else:
                src = g.xres[bi, col0:col0 + Lu, :]
            bcast_row(g, "a1row", g.modv, (r * 6 + 1) * D, D, t=a1)
            bcast_row(g, "s1row", g.modv, (r * 6 + 0) * D, D, t=s1)
            for t in range(Lu // 128):
                x_ = xt[t % 2]
                b.dma(x_, src[t * 128:(t + 1) * 128, :])
                b.act(junk, x_, AF.Square, accum_out=ss)
                b.ts(ss, ss, 1.0 / D, EPS, op0=ALU.mult, op1=ALU.add)
                b.act(ss, ss, AF.Sqrt)
                b.recip(ss, ss)
                b.stt(junk, x_, ss, a1, ALU.mult, ALU.mult)
                h_ = hb[t % 2]
                b.tt(h_, junk, s1, ALU.add)
                pt = g.pbb[t % 2]
                for kd in range(KD):
                    b.tr(pt[:, kd * 128:(kd + 1) * 128], h_[:, kd * 128:(kd + 1) * 128], g.identb)
                b.copy(hT[:, :, t * 128:(t + 1) * 128], pt[:, 0:KD * 128].rearrange("p (k t) -> p k t", k=KD), e="act")
            NBLK = min(512, Lu)
            fm_ranges = [(c.off[0], c.off[2]), (c.off[5], c.off[10]), (c.off[12], c.off[13])]
            fm_chunks = [(m0, min(128, r1 - m0)) for (r0_, r1) in fm_ranges for m0 in range(r0_, r1, 128)]
            for (m0, mw) in fm_chunks:
                for nb in range(Lu // NBLK):
                    pp = g.pb[nst % 2]
                    for kd in range(KD):
                        b.mm(pp[0:mw, 0:NBLK], wbf[:, kd, m0:m0 + mw], hT[:, kd, nb * NBLK:(nb + 1) * NBLK], start=(kd == 0), stop=(kd == KD - 1))
                    s_ = stg[nst % 2]
                    b.copy(s_[0:mw, 0:NBLK], pp[0:mw, 0:NBLK], e=("act" if nst % 2 else "dve"))
                    b.dma(g.zT[bi, m0:m0 + mw, col0 + nb * NBLK: col0 + (nb + 1) * NBLK], s_[0:mw, 0:NBLK])
                    nst += 1
            for t in range(Lu // 128):
                s2 = stg2[t % 2]
                tm_ranges = [(c.off[2], c.off[5]), (c.off[10], c.off[12]), (c.off[13], DIN)]
                tm_chunks = [(c0, min(512, r1 - c0)) for (r0_, r1) in tm_ranges for c0 in range(r0_, r1, 512)]
                for (c0, cw) in tm_chunks:
                    pp = g.pb[2 + nst % 2]
                    for kd in range(KD):
                        b.mm(pp[:, 0:cw], hT[:, kd, t * 128:(t + 1) * 128], wbf[:, kd, c0:c0 + cw], start=(kd == 0), stop=(kd == KD - 1))
                    b.copy(s2[:, c0:c0 + cw], pp[:, 0:cw], e=("act" if nst % 2 else "dve"))
                    nst += 1
                for (r0_, r1) in tm_ranges:
                    b.dma(g.zt[bi, col0 + t * 128: col0 + (t + 1) * 128, r0_:r1], s2[:, r0_:r1])


def colparam(g, t, j, src, off, n):
    g.b.dma(t[0:n, j:j + 1], mkap(src, off, [[1, n], [1, 1]]))


def nblocks(n, bs=512):
    out, s = [], 0
    while s < n:
        out.append((s, min(bs, n - s)))
        s += bs
    return out


def gelu_(b, out, x, t1, t2):
    b.tt(t1, x, x, ALU.mult)
    b.ts(t1, t1, 0.044715, 1.0, op0=ALU.mult, op1=ALU.add)
    b.tt(t1, t1, x, ALU.mult)
    b.act(t2, t1, AF.Sigmoid, scale=1.5957691216057308)
    b.tt(out, x, t2, ALU.mult)


def to_tokmajor(g, src, PW, n, dst_fn, nm="tk"):
    b = g.b
    stg = [b.sb("%s_stg%d" % (nm, i), [128, PW]) for i in range(2)]
    for t in range(n // 128):
        pp = g.pb[4 + t % 2]
        b.tr(pp[:, 0:PW], src[0:PW, t * 128:(t + 1) * 128], g.ident[0:PW, 0:PW])
        s_ = stg[t % 2]
        b.copy(s_, pp[:, 0:PW], e=("act" if t % 2 else "dve"))
        dst_fn(t, s_)


def stage_lru(g, l):
    b, c = g.b, g.c
    W, LT, LC, L = c.W, c.LT, c.LC, c.L
    PW = min(128, W)
    hp = PW // 64
    for ct in range(W // PW):
        b.push()
        ch0 = ct * PW
        pc = b.sb("lru_pc", [PW, 16])
        for j in range(4):
            colparam(g, pc, j, g.w["lru_conv_w"], (l * 4 + j) * W + ch0, PW)
        colparam(g, pc, 4, g.w["lru_conv_b"], l * W + ch0, PW)
        for d in range(2):
            colparam(g, pc, 5 + d, g.w["lru_br"], (l * 2 + d) * W + ch0, PW)
            colparam(g, pc, 7 + d, g.w["lru_bi"], (l * 2 + d) * W + ch0, PW)
            colparam(g, pc, 9 + d, g.w["lru_lam"], (l * 2 + d) * W + ch0, PW)
        b.act(pc[:, 11:13], pc[:, 9:11], AF.Exp, scale=-1.0)
        b.act(pc[:, 11:13], pc[:, 11:13], AF.Ln, bias=1.0)
        b.ts(pc[:, 11:13], pc[:, 11:13], -8.0, None, op0=ALU.mult)
        wbd = {}
        for d in range(2):
            for gi, gn in enumerate(("lru_wr", "lru_wi")):
                wt = b.sb("lru_w%d%d" % (d, gi), [PW, PW])
                b.memset(wt, 0.0)
                for h in range(hp):
                    b.dma(wt[h * 64:(h + 1) * 64, h * 64:(h + 1) * 64], g.w[gn][l, d, ct * hp + h])
                wbd[(d, gi)] = wt
        xz, gz, xc, r_, i_, a_, bb, h0, h1 = [b.sb("lru_t%d" % i, [PW, LT]) for i in range(9)]
        for bi in range(c.NB):
            b.dma(xz, g.zT[bi, c.off[0] + ch0: c.off[0] + ch0 + PW, :])
            b.dma(gz, g.zT[bi, c.off[1] + ch0: c.off[1] + ch0 + PW, :])
            b.ts(xc, xz, pc[:, 2:3], pc[:, 4:5], op0=ALU.mult, op1=ALU.add)
            for (s0, n) in ((0, LC), (LC, L)):
                for j in (0, 1, 3):
                    sh = j - 2
                    if sh < 0:
                        o_ = xc[:, s0 - sh: s0 + n]
                        i0 = xz[:, s0: s0 + n + sh]
                    else:
                        o_ = xc[:, s0: s0 + n - sh]
                        i0 = xz[:, s0 + sh: s0 + n]
                    b.stt(o_, i0, pc[:, j:j + 1], o_, ALU.mult, ALU.add)
            for d in range(2):
                for (s0, n) in nblocks(LT):
                    pp = g.pb[0]
                    b.mm(pp[0:PW, 0:n], wbd[(d, 0)], xc[:, s0:s0 + n])
                    b.act(r_[:, s0:s0 + n], pp[0:PW, 0:n], AF.Sigmoid, bias=pc[:, 5 + d:6 + d])
                    pp = g.pb[1]
                    b.mm(pp[0:PW, 0:n], wbd[(d, 1)], xc[:, s0:s0 + n])
                    b.act(i_[:, s0:s0 + n], pp[0:PW, 0:n], AF.Sigmoid, bias=pc[:, 7 + d:8 + d])
                b.act(a_, r_, AF.Exp, scale=pc[:, 11 + d:12 + d])
                b.tt(bb, a_, a_, ALU.mult)
                b.act(bb, bb, AF.Sqrt, scale=-1.0, bias=1.0)
                b.tt(bb, bb, i_, ALU.mult)
                b.tt(bb, bb, xc, ALU.mult)
                if d == 0:
                    b.scan(h0, a_, bb, 0.0)
                else:
                    b.scan(rev(h1[:, 0:LC]), rev(a_[:, 0:LC]), rev(bb[:, 0:LC]), 0.0)
                    b.scan(rev(h1[:, LC:LT]), rev(a_[:, LC:LT]), rev(bb[:, LC:LT]), h1[:, 0:1])
            b.tt(h0, h0, h1, ALU.add)
            gelu_(b, r_, gz, i_, a_)
            b.tt(h0, h0, r_, ALU.mult)
            to_tokmajor(g, h0, PW, LT, lambda t, s_: b.dma(g.ymix[bi, t * 128:(t + 1) * 128, ch0:ch0 + PW], s_), nm="lru")
        b.pop()


def conv_seg(b, out, x, pc, taps, pad_left, segs, bias_col):
    ctr = pad_left
    b.ts(out, x, pc[:, ctr:ctr + 1], bias_col, op0=ALU.mult, op1=ALU.add)
    for (s0, n) in segs:
        for j in range(taps):
            sh = j - pad_left
            if sh == 0:
                continue
            if sh < 0:
                o_ = out[:, s0 - sh: s0 + n]
                i0 = x[:, s0: s0 + n + sh]
            else:
                o_ = out[:, s0: s0 + n - sh]
                i0 = x[:, s0 + sh: s0 + n]
            b.stt(o_, i0, pc[:, j:j + 1], o_, ALU.mult, ALU.add)


def stage_mlstm(g, l):
    b, c = g.b, g.c
    W, H, LT, LC, L, GW = c.W, c.H, c.LT, c.LC, c.L, c.GW
    R = L // GW
    NT = LT // 128
    NTC = LC // 128
    b.push()
    io = b.sb("ml_io", [128, 128])
    b.k.op("pool", lambda E: E.iota(io, pattern=[[1, 128]], base=0, channel_multiplier=-1, allow_small_or_imprecise_dtypes=True), [], [key(io)])
    tri = [b.sb("ml_tri%d" % d, [128, 128]) for d in range(2)]
    b.ts(tri[0], io, 0.0, None, op0=ALU.is_ge)
    b.ts(tri[1], io, 0.0, None, op0=ALU.is_le)
    ones = b.sb("ml_ones", [H, LT])
    zeros = b.sb("ml_zeros", [H, LT])
    b.memset(ones, 1.0)
    b.memset(zeros, 0.0)
    gb = b.sb("ml_gb", [H, 8])
    for d in range(2):
        for f in range(2):
            colparam(g, gb, d * 2 + f, g.w["mlstm_gate_b"], ((l * 2 + d) * 2 + f) * H, H)
    b.ts(gb[:, 4:8], gb[:, 0:4], -1.0, None, op0=ALU.mult)
    pcq = [b.sb("ml_pc%d" % i, [64, 8]) for i in range(2 * H)]
    for qk in range(2):
        for h in range(H):
            t = pcq[qk * H + h]
            ch = qk * W + h * 64
            for j in range(4):
                colparam(g, t, j, g.w["mlstm_conv_w"], (l * 4 + j) * 2 * W + ch, 64)
            colparam(g, t, 4, g.w["mlstm_conv_b"], l * 2 * W + ch, 64)
    qT1 = b.sb("ml_q", [64, LT], BF16)
    kT1 = b.sb("ml_k", [64, LT], BF16)
    qT = [qT1] * H
    kT = [kT1] * H
    raw = b.sb("ml_raw", [64, LT])
    raw2 = b.sb("ml_raw2", [64, LT])
    igt = b.sb("ml_ig", [H, LT])
    fgt = b.sb("ml_fg", [H, LT])
    tmpg = b.sb("ml_tmpg", [H, LT])
    Lc = b.sb("ml_Lc", [H, LT])
    u_ = b.sb("ml_u", [H, LT])
    P_ = b.sb("ml_P", [H, LT])
    WB = [b.sb("ml_WB%d" % d, [128, LT]) for d in range(2)]
    ucol = b.sb("ml_ucol", [128, 2, NT])
    vaugb = b.sb("ml_vaugb", [128, NT, 65], BF16)
    emc = b.sb("ml_emc", [128, 2 * H, NT])
    Ex = [b.sb("ml_Ex%d" % i, [128, 512]) for i in range(2)]
    Sm = [b.sb("ml_Sm%d" % i, [128, 512], BF16) for i in range(2)]
    vaug = b.sb("ml_vaug", [128, NT, 65])
    osg = b.sb("ml_osg", [128, 64])
    hacc = b.sb("ml_hacc", [128, 4, 64])
    nd = b.sb("ml_nd", [128, 65])
    dm = b.sb("ml_dm", [128, 1])
    segs = ((0, LC), (LC, L))

    def cm(t_out, t_in, rows):
        b.copy(t_out[0:rows, 0:LC], t_in[0:rows, 0:LC], e="pool")
        b.copy(fview(t_out[0:rows, :], [[R, GW], [1, R]], off=LC), fview(t_in[0:rows, :], [[1, GW], [GW, R]], off=LC))

    def tokrows(dr, bi, t, c0, n):
        rs = dr.ap[1][0]
        base = dr[bi].offset
        if t < NTC:
            return mkap(dr, base + (t * 128) * rs + c0, [[rs, 128], [1, n]])
        n0 = (t - NTC) * 128
        w0 = n0 // R
        nw = 128 // R
        return mkap(dr, base + (LC + w0) * rs + c0, [[rs, nw], [GW * rs, R], [1, n]])

    for bi in range(c.NB):
        for d in range(2):
            r0 = c.off[12] + d * 2 * H
            b.dma(tmpg, g.zT[bi, r0:r0 + H, :])
            cm(igt, tmpg, H)
            b.dma(tmpg, g.zT[bi, r0 + H:r0 + 2 * H, :])
            cm(fgt, tmpg, H)
            b.ts(igt, igt, gb[:, d * 2:d * 2 + 1], None, op0=ALU.add)
            b.act(fgt, fgt, AF.Exp, scale=-1.0, bias=gb[:, 4 + d * 2 + 1: 4 + d * 2 + 2])
            b.act(fgt, fgt, AF.Ln, bias=1.0)
            if d == 0:
                b.scan(Lc, ones, fgt, 0.0)
            else:
                b.scan(rev(Lc[:, 0:LC]), rev(ones[:, 0:LC]), rev(fgt[:, 0:LC]), 0.0)
                b.scan(rev(Lc[:, LC:LT]), rev(ones[:, LC:LT]), rev(fgt[:, LC:LT]), Lc[:, 0:1])
            b.tt(u_, igt, Lc, ALU.add)
            if d == 0:
                b.scan(P_, u_, zeros, 0.0, op0=ALU.max, op1=ALU.max)
            else:
                b.scan(rev(P_[:, 0:LC]), rev(u_[:, 0:LC]), rev(zeros[:, 0:LC]), 0.0, op0=ALU.max, op1=ALU.max)
                b.scan(rev(P_[:, LC:LT]), rev(u_[:, LC:LT]), rev(zeros[:, LC:LT]), P_[:, 0:1], op0=ALU.max, op1=ALU.max)
            b.tt(tmpg, Lc, P_, ALU.subtract)
            b.act(tmpg, tmpg, AF.Exp)
            b.ts(P_, P_, -1.0, None, op0=ALU.mult)
            b.dma(g.mlsc2[0, d], u_)
            b.dma(g.mlsc2[1, d], P_)
            b.dma(g.mlsc[d], tmpg)
            for h in range(H):
                b.dma(emc[:, d * H + h, :], mkap(g.mlsc, (d * H + h) * LT, [[1, 128], [128, NT]]), allow_slow_non_contiguous=True)
        for h in range(H):
            for qk in range(2):
                row0 = c.off[8 + qk] + h * 64
                b.dma(raw, g.zT[bi, row0:row0 + 64, :])
                cm(raw2, raw, 64)
                dst = (qT if qk == 0 else kT)[h]
                pc = pcq[qk * H + h]
                conv_seg(b, raw, raw2, pc, 4, 2, segs, pc[:, 4:5])
                b.act(raw, raw, AF.Silu)
                b.ts(dst, raw, (0.125 if qk == 0 else 1.0), None, op0=ALU.mult)
            for d in range(2):
                b.dma(WB[d], mkap(g.mlsc2, ((1 * 2 + d) * H + h) * LT, [[0, 128], [1, LT]]))
                b.dma(ucol[:, d, :], mkap(g.mlsc2, ((0 * 2 + d) * H + h) * LT, [[1, 128], [128, NT]]), allow_slow_non_contiguous=True)
            b.memset(vaug, 1.0)
            for t in range(NT):
                b.dma(vaug[:, t, 0:64], tokrows(g.zt, bi, t, c.off[10] + h * 64, 64))
            b.copy(vaugb, vaug)
            def Jlist(I, d):
                if d == 0:
                    return list(range(0, I + 1))
                if I < NTC:
                    return list(range(I, NTC))
                return list(range(0, NTC)) + list(range(I, NT))
            QBS = 4
            blocks = [list(range(i0, min(i0 + QBS, NTC))) for i0 in range(0, NTC, QBS)] + \
                     [list(range(i0, min(i0 + QBS, NT))) for i0 in range(NTC, NT, QBS)]
            for QB in blocks:
                nq = len(QB)
                c0q, c1q = QB[0] * 128, (QB[-1] + 1) * 128
                for d in range(2):
                    jl = {I: Jlist(I, d) for I in QB}
                    Jall = sorted(set(j for I in QB for j in jl[I]))
                    for ji, J in enumerate(Jall):
                        ps_s = g.pb[ji % 2]
                        b.mm(ps_s[:, 0:nq * 128], kT[h][:, J * 128:(J + 1) * 128], qT[h][:, c0q:c1q])
                        ex = Ex[ji % 2]
                        sm = Sm[ji % 2]
                        b.act(ex[:, 0:nq * 128], WB[d][:, c0q:c1q], AF.Exp, bias=ucol[:, d, J:J + 1])
                        if J in QB:
                            qi = QB.index(J)
                            b.tt(ex[:, qi * 128:(qi + 1) * 128], ex[:, qi * 128:(qi + 1) * 128], tri[d], ALU.mult, e="pool")
                        b.tt(sm[:, 0:nq * 128], ps_s[:, 0:nq * 128], ex[:, 0:nq * 128], ALU.mult)
                        for qi, I in enumerate(QB):
                            if J in jl[I]:
                                b.mm(g.pb[2 + qi][:, 0:65], sm[:, qi * 128:(qi + 1) * 128], vaugb[:, J, :], start=(J == jl[I][0]), stop=(J == jl[I][-1]))
                    for qi, I in enumerate(QB):
                        b.copy(nd, g.pb[2 + qi][:, 0:65], e="act")
                        b.stt(dm, nd[:, 64:65], -1.0, nd[:, 64:65], ALU.mult, ALU.max)
                        b.ts(dm, dm, emc[:, d * H + h, I:I + 1], None, op0=ALU.max)
                        b.recip(dm, dm)
                        if d == 0:
                            b.ts(hacc[:, qi, :], nd[:, 0:64], dm, None, op0=ALU.mult)
                        else:
                            b.stt(hacc[:, qi, :], nd[:, 0:64], dm, hacc[:, qi, :], ALU.mult, ALU.add)
                for qi, I in enumerate(QB):
                    b.dma(osg, tokrows(g.zt, bi, I, c.off[11] + h * 64, 64))
                    b.act(osg, osg, AF.Sigmoid)
                    b.tt(osg, osg, hacc[:, qi, :], ALU.mult)
                    b.dma(tokrows(g.ymix, bi, I, 2 * W + h * 64, 64), osg)
    b.pop()


def hy_consts(c):
    import ml_dtypes
    out = {}
    for nm, Lu in (("c", c.LC), ("l", c.L)):
        n = np.arange(Lu, dtype=np.float64)
        f = np.arange(Lu, dtype=np.float64) + 0.5
        ang = np.pi * np.outer(n, f) / Lu
        out["hyCcf_" + nm] = np.cos(ang).astype(ml_dtypes.bfloat16)
        out["hyCsf_" + nm] = np.sin(ang).astype(ml_dtypes.bfloat16)
        out["hyCci_" + nm] = np.ascontiguousarray(np.cos(ang).T).astype(ml_dtypes.bfloat16)
        out["hyCsi_" + nm] = np.ascontiguousarray(np.sin(ang).T).astype(ml_dtypes.bfloat16)
        pos = np.arange(Lu, dtype=np.float32)
        t = pos / np.float32(Lu - 1)
        freqs = np.linspace(1e-4, 15, 16, dtype=np.float32)
        a2 = (np.float32(2 * np.pi / Lu) * pos[:, None] * freqs[None, :]).astype(np.float32)
        z = np.concatenate([t[:, None], np.cos(a2), -np.sin(a2)], -1).astype(np.float32)
        out["hyzfT_" + nm] = np.ascontiguousarray(z.T)
        out["hytcol_" + nm] = np.ascontiguousarray((-t).reshape(Lu // 128, 128).T)
    out["hydelta"] = np.abs(np.linspace(np.log(1e-2) / 1.5, np.log(1e-2) / 0.3, c.W, dtype=np.float32)).reshape(1, c.W)
    return out


def hy_decl(g):
    b, c = g.b, g.c
    g.hyc = {}
    for nm, Lu in (("c", c.LC), ("l", c.L)):
        for k_ in ("Ccf", "Csf", "Cci", "Csi"):
            g.hyc[k_ + nm] = b.dram("hy%s_%s" % (k_, nm), [Lu, Lu], BF16, kind="ExternalInput")
        g.hyc["zfT" + nm] = b.dram("hyzfT_" + nm, [33, Lu], kind="ExternalInput")
        g.hyc["tcol" + nm] = b.dram("hytcol_" + nm, [128, Lu // 128], kind="ExternalInput")
    g.hyc["delta"] = b.dram("hydelta", [1, c.W], kind="ExternalInput")
    g.hyu = b.dram("hyu", [c.LT, c.NB, 3 * c.W])
    g.hyz1 = b.dram("hyz1", [c.L, c.NB, c.W])


def stage_hyena(g, l):
    b, c = g.b, g.c
    W, LT, LC, L, NB = c.W, c.LT, c.LC, c.L, c.NB
    NBW = NB * W
    W3 = 3 * W
    PI = float(np.pi)
    b.push()
    cw = [b.sb("hy_cw%d" % j, [128, W3]) for j in range(3)]
    for j in range(3):
        b.dma(cw[j], mkap(g.w["hy_conv_w"], (l * 3 + j) * W3, [[0, 128], [1, W3]]))
    cb = b.sb("hy_cb", [128, W3])
    b.dma(cb, mkap(g.w["hy_conv_b"], l * W3, [[0, 128], [1, W3]]))
    cur = [b.sb("hy_cur%d" % i, [128, W3]) for i in range(2)]
    prv = [b.sb("hy_prv%d" % i, [128, W3]) for i in range(2)]
    nxt = [b.sb("hy_nxt%d" % i, [128, W3]) for i in range(2)]
    acc = [b.sb("hy_acc%d" % i, [128, W3]) for i in range(2)]
    it = 0
    for bi in range(NB):
        for (s0, n) in ((0, LC), (LC, L)):
            for t in range(n // 128):
                r0 = s0 + t * 128
                cu, pr, nx, ac = cur[it % 2], prv[it % 2], nxt[it % 2], acc[it % 2]
                it += 1
                c0 = c.off[13]
                b.dma(cu, g.zt[bi, r0:r0 + 128, c0:c0 + W3])
                if t == 0:
                    b.memset(pr, 0.0)
                    b.dma(pr[1:128, :], g.zt[bi, r0:r0 + 127, c0:c0 + W3])
                else:
                    b.dma(pr, g.zt[bi, r0 - 1:r0 + 127, c0:c0 + W3])
                if t == n // 128 - 1:
                    b.memset(nx, 0.0, e="pool")
                    b.dma(nx[0:127, :], g.zt[bi, r0 + 1:r0 + 128, c0:c0 + W3])
                else:
                    b.dma(nx, g.zt[bi, r0 + 1:r0 + 129, c0:c0 + W3])
                b.tt(ac, cu, cw[1], ALU.mult)
                b.tt(ac, ac, cb, ALU.add)
                b.tt(pr, pr, cw[0], ALU.mult, e="pool")
                b.tt(nx, nx, cw[2], ALU.mult, e="pool")
                b.tt(ac, ac, pr, ALU.add)
                b.tt(ac, ac, nx, ALU.add)
                b.dma(g.hyu[r0:r0 + 128, bi, :], ac)
    b.pop()
    for nm, s0, Lu in (("c", 0, LC), ("l", LC, L)):
        if nm == "c" and l == c.depth - 1 and not getattr(g, "force_ctx", False):
            continue
        NT = Lu // 128
        b.push()
        Ccf, Csf, Cci, Csi = [g.hyc[k_ + nm] for k_ in ("Ccf", "Csf", "Cci", "Csi")]
        zf = b.sb("hy_zf", [33, Lu])
        b.dma(zf, g.hyc["zfT" + nm])
        w1 = b.sb("hy_w1", [33, 64]); b.dma(w1, g.w["hy_w1"][l])
        w2 = b.sb("hy_w2", [64, 64]); b.dma(w2, g.w["hy_w2"][l])
        w3 = b.sb("hy_w3", [64, 4 * W]); b.dma(w3, g.w["hy_w3"][l])
        pcol = b.sb("hy_pcol", [64, 4])
        colparam(g, pcol, 0, g.w["hy_b1"], l * 64, 64)
        colparam(g, pcol, 1, g.w["hy_b2"], l * 64, 64)
        colparam(g, pcol, 2, g.w["hy_freq"], (l * 2 + 0) * 64, 64)
        colparam(g, pcol, 3, g.w["hy_freq"], (l * 2 + 1) * 64, 64)
        h1 = b.sb("hy_h1", [64, Lu])
        h2 = b.sb("hy_h2", [64, Lu])
        hcnt = b.sb("hy_hcnt", [64, Lu])
        for (wt, src, dst, bcol, fcol, K) in ((w1, zf, h1, 0, 2, 33), (w2, h1, h2, 1, 3, 64)):
            for (n0, n) in nblocks(Lu):
                pp = g.pb[0]
                b.mm(pp[0:64, 0:n], wt[0:K, :], src[0:K, n0:n0 + n])
                b.ts(dst[:, n0:n0 + n], pp[0:64, 0:n], pcol[:, bcol:bcol + 1], pcol[:, fcol:fcol + 1], op0=ALU.add, op1=ALU.mult)
            b.ts(dst, dst, 11.0 * PI, None, op0=ALU.add)
            b.memset(hcnt, 0.0)
            for m_ in range(1, 11):
                b.stt(hcnt, dst, 2.0 * PI * m_, hcnt, ALU.is_ge, ALU.add)
            b.stt(dst, hcnt, -2.0 * PI, dst, ALU.mult, ALU.add)
            b.act(dst, dst, AF.Sin, bias=g.negpi[0:64, :])
        AB = b.sb("hy_AB", [128, NT, 2, 2 * W], BF16)
        tcol = b.sb("hy_tcol", [128, NT]); b.dma(tcol, g.hyc["tcol" + nm])
        drow = b.sb("hy_drow", [128, W]); b.dma(drow, mkap(g.hyc["delta"], 0, [[0, 128], [1, W]]))
        ones = b.sb("hy_ones", [128, 128]); b.memset(ones, 1.0)
        dec = b.sb("hy_dec", [128, W])
        tp = b.sb("hy_tp", [128, 4 * W])
        ab = b.sb("hy_abs", [128, 4 * W])
        psum_abs = [g.pb[4], g.pb[5]]
        HW = 2 * W
        for t in range(NT):
            for hf in range(2):
                pp = g.pb[hf]
                b.mm(pp[:, 0:HW], h2[:, t * 128:(t + 1) * 128], w3[:, hf * HW:(hf + 1) * HW])
            b.act(dec, drow, AF.Exp, scale=tcol[:, t:t + 1])
            for hf in range(2):
                b.tt(tp[:, hf * HW:(hf + 1) * HW].rearrange("p (d c) -> p d c", d=2), g.pb[hf][:, 0:HW].rearrange("p (d c) -> p d c", d=2),
                     fview(dec, [[0, 2], [1, W]]), ALU.mult)
            if t == 0:
                for o in range(2):
                    b.memset(tp[0:1, o * HW + W:(o + 1) * HW], 0.0)
            b.stt(ab, tp, -1.0, tp, ALU.mult, ALU.max)
            for o in range(2):
                b.mm(psum_abs[o][:, 0:HW], ones, ab[:, o * HW:(o + 1) * HW], start=(t == 0), stop=(t == NT - 1))
                fw = tp[:, o * HW: o * HW + W]
                bw = tp[:, o * HW + W: (o + 1) * HW]
                b.tt(AB[:, t, 0, o * W:(o + 1) * W], fw, bw, ALU.add)
                b.tt(AB[:, t, 1, o * W:(o + 1) * W], fw, bw, ALU.subtract)
        rn = b.sb("hy_rn", [128, 2 * W])
        for o in range(2):
            b.copy(rn[:, o * W:(o + 1) * W], psum_abs[o][:, 0:W], e="act")
            b.tt(rn[:, o * W:(o + 1) * W], rn[:, o * W:(o + 1) * W], psum_abs[o][:, W:HW], ALU.add)
        b.ts(rn, rn, EPS, None, op0=ALU.add)
        b.recip(rn, rn)
        G = b.sb("hy_G", [128, NT, 2, 2 * W], BF16)
        blk = [b.sb("hy_blk%d" % i, [128, NT, 128], BF16) for i in range(4)]
        nb_ = 0
        for ft in range(NT):
            bc, bs = blk[(nb_ * 2) % 4], blk[(nb_ * 2 + 1) % 4]
            nb_ += 1
            b.dma(bc, Ccf[:, ft * 128:(ft + 1) * 128].rearrange("(t p) f -> p t f", p=128))
            b.dma(bs, Csf[:, ft * 128:(ft + 1) * 128].rearrange("(t p) f -> p t f", p=128), q="pool")
            for nt in range(NT):
                b.mm(g.pb[0][:, 0:2 * W], bc[:, nt, :], AB[:, nt, 0, :], start=(nt == 0), stop=(nt == NT - 1))
            for nt in range(NT):
                b.mm(g.pb[1][:, 0:2 * W], bs[:, nt, :], AB[:, nt, 1, :], start=(nt == 0), stop=(nt == NT - 1))
            b.tt(G[:, ft, 0, :], g.pb[0][:, 0:2 * W], rn, ALU.mult)
            b.stt(G[:, ft, 1, :], g.pb[1][:, 0:2 * W], -1.0, rn, ALU.mult, ALU.mult)
        X = b.sb("hy_X", [128, NT, NBW], BF16)
        Pre = b.sb("hy_Pre", [128, NT, NBW], BF16)
        Pin = b.sb("hy_Pin", [128, NT, NBW], BF16)
        ut = [b.sb("hy_ut%d" % i, [128, NB, W3]) for i in range(2)]
        z1t = [b.sb("hy_z1t%d" % i, [128, NB, W]) for i in range(2)]
        t1 = b.sb("hy_t1", [128, NBW])
        t2 = b.sb("hy_t2", [128, NBW])
        brow = [b.sb("hy_brow%d" % o, [128, W]) for o in range(2)]
        for o in range(2):
            b.dma(brow[o], mkap(g.w["hy_bias"], (l * 2 + o) * W, [[0, 128], [1, W]]))
        for t in range(NT):
            u_ = ut[t % 2]
            b.dma(u_, g.hyu[s0 + t * 128: s0 + (t + 1) * 128])
            b.copy(X[:, t, :].rearrange("p (b c) -> p b c", b=NB), u_[:, :, 0:W])
        for o in range(2):
            for ft in range(NT):
                bc, bs = blk[(nb_ * 2) % 4], blk[(nb_ * 2 + 1) % 4]
                nb_ += 1
                b.dma(bc, Ccf[:, ft * 128:(ft + 1) * 128].rearrange("(t p) f -> p t f", p=128))
                b.dma(bs, Csf[:, ft * 128:(ft + 1) * 128].rearrange("(t p) f -> p t f", p=128), q="pool")
                ure, uim = g.pb[0], g.pb[1]
                for st_ in range(NT):
                    b.mm(ure[:, 0:NBW], bc[:, st_, :], X[:, st_, :], start=(st_ == 0), stop=(st_ == NT - 1))
                for st_ in range(NT):
                    b.mm(uim[:, 0:NBW], bs[:, st_, :], X[:, st_, :], start=(st_ == 0), stop=(st_ == NT - 1))
                gre = fview(G[:, ft, 0, o * W:(o + 1) * W], [[0, NB], [1, W]])
                gim = fview(G[:, ft, 1, o * W:(o + 1) * W], [[0, NB], [1, W]])
                v3 = lambda a: a.rearrange("p (b c) -> p b c", b=NB)
                b.tt(v3(t1), v3(ure[:, 0:NBW]), gre, ALU.mult)
                b.tt(v3(t2), v3(uim[:, 0:NBW]), gim, ALU.mult)
                b.tt(Pre[:, ft, :], t1, t2, ALU.add)
                b.tt(v3(t1), v3(uim[:, 0:NBW]), gre, ALU.mult)
                b.tt(v3(t2), v3(ure[:, 0:NBW]), gim, ALU.mult)
                b.tt(Pin[:, ft, :], t1, t2, ALU.subtract)
            for tt_ in range(NT):
                bc, bs = blk[(nb_ * 2) % 4], blk[(nb_ * 2 + 1) % 4]
                nb_ += 1
                b.dma(bc, Cci[:, tt_ * 128:(tt_ + 1) * 128].rearrange("(t p) f -> p t f", p=128))
                b.dma(bs, Csi[:, tt_ * 128:(tt_ + 1) * 128].rearrange("(t p) f -> p t f", p=128), q="pool")
                yp = g.pb[2 + tt_ % 2]
                for ft in range(NT):
                    b.mm(yp[:, 0:NBW], bc[:, ft, :], Pre[:, ft, :], start=(ft == 0), stop=False)
                for ft in range(NT):
                    b.mm(yp[:, 0:NBW], bs[:, ft, :], Pin[:, ft, :], start=False, stop=(ft == NT - 1))
                u_ = ut[tt_ % 2]
                b.dma(u_, g.hyu[s0 + tt_ * 128: s0 + (tt_ + 1) * 128])
                z_ = z1t[tt_ % 2]
                v3 = lambda a: a.rearrange("p (b c) -> p b c", b=NB)
                bro = fview(brow[o], [[0, NB], [1, W]])
                if o == 0:
                    b.tt(v3(t1), u_[:, :, 0:W], bro, ALU.mult)
                    b.stt(t1, yp[:, 0:NBW], 1.0 / Lu, t1, ALU.mult, ALU.add)
                    b.tt(z_, v3(t1), u_[:, :, W:2 * W], ALU.mult)
                    b.copy(X[:, tt_, :].rearrange("p (b c) -> p b c", b=NB), z_, e="pool")
                    b.dma(g.hyz1[tt_ * 128:(tt_ + 1) * 128], z_)
                else:
                    b.dma(z_, g.hyz1[tt_ * 128:(tt_ + 1) * 128])
                    b.tt(v3(t1), z_, bro, ALU.mult)
                    b.stt(t1, yp[:, 0:NBW], 1.0 / Lu, t1, ALU.mult, ALU.add)
                    b.tt(v3(t2), v3(t1), u_[:, :, 2 * W:3 * W], ALU.mult)
                    for bi in range(NB):
                        b.dma(g.ymix[bi, s0 + tt_ * 128: s0 + (tt_ + 1) * 128, 3 * W:4 * W], t2[:, bi * W:(bi + 1) * W])
        b.pop()


def stage_merge(g, l):
    b, c = g.b, g.c
    D, KD, W, LT, LC, L = c.D, c.KD, c.W, c.LT, c.LC, c.L
    need_ctx = (l < c.depth - 1) or getattr(g, "force_ctx", False)
    wob = b.sb("mg_wob", [128, KD, D], BF16)
    wst = [b.sb("mg_wst%d" % i, [128, D]) for i in range(2)]
    for kd in range(KD):
        b.dma(wst[kd % 2], g.w["w_out"][l, kd * 128:(kd + 1) * 128, :])
        b.copy(wob[:, kd, :], wst[kd % 2], e=("dve" if kd % 2 else "pool"))
    gg = b.sb("mg_gg", [128, D])
    b.dma(gg, mkap(g.w["grp_g"], l * D, [[0, 128], [1, D]]))
    gate = b.sb("mg_gate", [128, D])
    ym = [b.sb("mg_ym%d" % i, [128, D]) for i in range(2)]
    xt = [b.sb("mg_xt%d" % i, [128, D]) for i in range(2)]
    yb = [b.sb("mg_yb%d" % i, [128, D], BF16) for i in range(2)]
    yT = [b.sb("mg_yT%d" % i, [128, KD, 128], BF16) for i in range(2)]
    junk = b.sb("mg_junk", [128, W])
    ss = b.sb("mg_ss", [128, 4])
    it = 0
    for bi in range(c.NB):
        for seg in range(2):
            if seg == 0 and not need_ctx:
                continue
            r = 2 if seg == 0 else bi
            Lu = LC if seg == 0 else L
            s0 = 0 if seg == 0 else LC
            b.dma(gate, mkap(g.modv, (r * 6 + 2) * D, [[0, 128], [1, D]]))
            for t in range(Lu // 128):
                r0 = s0 + t * 128
                y_, x_, yb_, yT_ = ym[it % 2], xt[it % 2], yb[it % 2], yT[it % 2]
                it += 1
                b.dma(y_, g.ymix[bi, r0:r0 + 128, :])
                if l == 0:
                    b.dma(x_, (g.ctx[bi, t * 128:(t + 1) * 128, :] if seg == 0 else g.x[bi, t * 128:(t + 1) * 128, :]), q="pool")
                else:
                    b.dma(x_, g.xres[bi, r0:r0 + 128, :], q="pool")
                for q_ in range(4):
                    b.act(junk, y_[:, q_ * W:(q_ + 1) * W], AF.Square, accum_out=ss[:, q_:q_ + 1])
                b.ts(ss, ss, 1.0 / W, EPS, op0=ALU.mult, op1=ALU.add)
                b.act(ss, ss, AF.Sqrt)
                b.recip(ss, ss)
                for q_ in range(4):
                    b.stt(yb_[:, q_ * W:(q_ + 1) * W], y_[:, q_ * W:(q_ + 1) * W], ss[:, q_:q_ + 1], gg[:, q_ * W:(q_ + 1) * W], ALU.mult, ALU.mult)
                pt = g.pbb[it % 2]
                for kd in range(KD):
                    b.tr(pt[:, kd * 128:(kd + 1) * 128], yb_[:, kd * 128:(kd + 1) * 128], g.identb)
                b.copy(yT_, pt[:, 0:KD * 128].rearrange("p (k t) -> p k t", k=KD), e="act")
                for hf in range((D + 511) // 512):
                    n0 = hf * 512
                    n = min(512, D - n0)
                    pp = g.pb[hf % 2]
                    for kd in range(KD):
                        b.mm(pp[:, 0:n], yT_[:, kd, :], wob[:, kd, n0:n0 + n], start=(kd == 0), stop=(kd == KD - 1))
                    b.tt(y_[:, n0:n0 + n], pp[:, 0:n], gate[:, n0:n0 + n], ALU.mult)
                b.tt(x_, x_, y_, ALU.add, e="pool")
                b.dma(g.xres[bi, r0:r0 + 128, :], x_)


def stage_final(g):
    b, c = g.b, g.c
    D, LC, L = c.D, c.LC, c.L
    gg = b.sb("fn_g", [128, D])
    b.dma(gg, mkap(g.w["final_g"], 0, [[0, 128], [1, D]]))
    xt = [b.sb("fn_x%d" % i, [128, D]) for i in range(2)]
    junk = b.sb("fn_junk", [128, D])
    ss = b.sb("fn_ss", [128, 1])
    it = 0
    for bi in range(c.NB):
        for t in range(L // 128):
            x_ = xt[it % 2]
            it += 1
            b.dma(x_, g.xres[bi, LC + t * 128: LC + (t + 1) * 128, :])
            b.act(junk, x_, AF.Square, accum_out=ss)
            b.ts(ss, ss, 1.0 / D, EPS, op0=ALU.mult, op1=ALU.add)
            b.act(ss, ss, AF.Sqrt)
            b.recip(ss, ss)
            b.stt(x_, x_, ss, gg, ALU.mult, ALU.mult)
            b.dma(g.out[bi, t * 128:(t + 1) * 128, :], x_)


def rw_decl(g):
    b, c = g.b, g.c
    g.puv16 = b.dram("puv16", [c.NK, 128, c.KD * 128 + c.D], BF16)
    g.rws = b.dram("rws", [c.NB, c.LT, 5, 2, c.W])
    g.vsd = b.dram("vsd", [c.NB, 2, c.W, c.LT])
    g.ysd = b.dram("ysd", [c.NB, 2, c.W, c.LT])
    g.rbon = b.dram("rbon", [c.NB, c.LT, c.W])
    g.rg = b.dram("rg", [c.NB, c.LT, c.W])


def stage_rwkv(g, l):
    b, c = g.b, g.c
    W, H, LT, LC, L, NB = c.W, c.H, c.LT, c.LC, c.L, c.NB
    W3 = 3 * W
    PW = min(128, W)
    NPW = W // PW
    segs = ((0, LC), (LC, L))
    b.push()
    def brow(name, src, off, n):
        t = b.sb(name, [128, n])
        b.dma(t, mkap(src, off, [[0, 128], [1, n]]))
        return t
    mu0 = b.sb("rw_mu0", [128, W3]); mu1 = b.sb("rw_mu1", [128, W3])
    for i in range(3):
        b.dma(mu0[:, i * W:(i + 1) * W], mkap(g.w["rwkv_mu"], ((l * 3 + i) * 2 + 0) * W, [[0, 128], [1, W]]))
        b.dma(mu1[:, i * W:(i + 1) * W], mkap(g.w["rwkv_mu"], ((l * 3 + i) * 2 + 1) * W, [[0, 128], [1, W]]))
    w0r = [brow("rw_w0%d" % d, g.w["rwkv_w0"], (l * 2 + d) * W, W) for d in range(2)]
    a0r = [brow("rw_a0%d" % d, g.w["rwkv_a0"], (l * 2 + d) * W, W) for d in range(2)]
    kkr = brow("rw_kkr", g.w["rwkv_kk"], l * W, W)
    kar = brow("rw_kar", g.w["rwkv_ka"], l * W, W)
    rkr = brow("rw_rkr", g.w["rwkv_rk"], l * W, W)
    lng = brow("rw_lng", g.w["rwkv_ln_g"], l * W, W)
    lnb = brow("rw_lnb", g.w["rwkv_ln_b"], l * W, W)
    w2 = [b.sb("rw_w2%d" % d, [32, W]) for d in range(2)]
    a2 = [b.sb("rw_a2%d" % d, [32, W]) for d in range(2)]
    for d in range(2):
        b.dma(w2[d], g.w["rwkv_w2"][l, d]); b.dma(a2[d], g.w["rwkv_a2"][l, d])
    g2 = b.sb("rw_g2", [64, W]); b.dma(g2, g.w["rwkv_g2"][l])
    def prep_set(i):
        return (b.sb("rw_cur%d" % i, [128, W3]), b.sb("rw_prv%d" % i, [128, W3]), b.sb("rw_nxt%d" % i, [128, W3]),
                b.sb("rw_lz%d" % i, [128, 128]), b.sb("rw_lza%d" % i, [32, 128]), b.sb("rw_lzg%d" % i, [64, 128]),
                b.sb("rw_kk%d" % i, [128, W]), b.sb("rw_t1%d" % i, [128, W]), b.sb("rw_t2%d" % i, [128, W]),
                b.sb("rw_a%d" % i, [128, W]), b.sb("rw_dec%d" % i, [128, W]), b.sb("rw_bon%d" % i, [128, W]),
                b.sb("rw_sh%d" % i, [128, H]), b.sb("rw_vT%d" % i, [PW, 128]))
    psets = [prep_set(0), prep_set(1)]
    pit = 0
    for bi in range(NB):
        for (s0, n) in segs:
            for t in range(n // 128):
                cur, prv, nxt, lz, lza, lzg, kk, t1, t2, a_, dec, bon, sh, vT = psets[pit % 2]
                pit += 1
                r0 = s0 + t * 128
                c0 = c.off[2]
                b.dma(cur, g.zt[bi, r0:r0 + 128, c0:c0 + W3])
                b.memset(prv, 0.0); b.memset(nxt, 0.0, e="pool")
                if t == 0:
                    b.dma(prv[1:128, :], g.zt[bi, r0:r0 + 127, c0:c0 + W3])
                else:
                    b.dma(prv, g.zt[bi, r0 - 1:r0 + 127, c0:c0 + W3])
                if t == n // 128 - 1:
                    b.dma(nxt[0:127, :], g.zt[bi, r0 + 1:r0 + 128, c0:c0 + W3])
                else:
                    b.dma(nxt, g.zt[bi, r0 + 1:r0 + 129, c0:c0 + W3])
                b.tt(prv, prv, cur, ALU.subtract); b.tt(prv, prv, mu0, ALU.mult)
                b.tt(nxt, nxt, cur, ALU.subtract, e="pool"); b.tt(nxt, nxt, mu1, ALU.mult, e="pool")
                b.tt(cur, cur, prv, ALU.add); b.tt(cur, cur, nxt, ALU.add)
                r_, k_, v_ = cur[:, 0:W], cur[:, W:2 * W], cur[:, 2 * W:3 * W]
                b.dma(lz, g.zT[bi, c.off[5]:c.off[5] + 128, r0:r0 + 128])
                b.dma(lza, g.zT[bi, c.off[6]:c.off[6] + 32, r0:r0 + 128])
                b.dma(lzg, g.zT[bi, c.off[7]:c.off[7] + 64, r0:r0 + 128])
                b.act(lz[0:32, :], lz[0:32, :], AF.Tanh)
                b.act(lzg, lzg, AF.Sigmoid)
                b.mm(g.pb[2][:, 0:W], lzg, g2)
                b.copy(t1, g.pb[2][:, 0:W], e="act")
                b.dma(g.rg[bi, r0:r0 + 128, :], t1)
                b.tt(kk, k_, kkr, ALU.mult)
                b.tt(t1, kk, kk, ALU.mult)
                b.reduce(sh, t1.rearrange("p (h k) -> p h k", h=H))
                b.act(sh, sh, AF.Sqrt)
                b.ts(sh, sh, 1e-12, None, op0=ALU.max)
                b.recip(sh, sh)
                b.tt(kk.rearrange("p (h k) -> p h k", h=H), kk.rearrange("p (h k) -> p h k", h=H), fview(sh, [[1, H], [0, 64]]), ALU.mult)
                for pw in range(NPW):
                    b.tr(g.pb[3][0:PW, 0:128], v_[:, pw * PW:(pw + 1) * PW], g.ident)
                    b.copy(vT, g.pb[3][0:PW, 0:128], e="act")
                    b.dma(g.vsd[bi, 0, pw * PW:(pw + 1) * PW, r0:r0 + 128], vT)
                    b.copy(vT, rev(g.pb[3][0:PW, 0:128]), e="act")
                    tlo = s0 + n - 128 - (r0 - s0)
                    b.dma(g.vsd[bi, 1, pw * PW:(pw + 1) * PW, tlo:tlo + 128], vT)
                for d in range(2):
                    b.mm(g.pb[0][:, 0:W], lz[0:32, :], w2[d])
                    b.mm(g.pb[1][:, 0:W], lza, a2[d])
                    b.tt(dec, g.pb[0][:, 0:W], w0r[d], ALU.add)
                    b.act(dec, dec, AF.Exp, scale=-1.0)
                    b.act(dec, dec, AF.Ln, bias=1.0)
                    b.act(dec, dec, AF.Exp, scale=-1.0, bias=g.neghalf)
                    b.act(dec, dec, AF.Exp, scale=-1.0)
                    b.tt(a_, g.pb[1][:, 0:W], a0r[d], ALU.add)
                    b.act(a_, a_, AF.Sigmoid)
                    b.stt(t1, a_, -1.0, kar, ALU.add, ALU.mult)
                    b.stt(t1, t1, 1.0, k_, ALU.add, ALU.mult)
                    b.tt(t2, kk, a_, ALU.mult)
                    rs = 5 * 2 * W
                    base = g.rws[bi].offset
                    def dst(comp):
                        return mkap(g.rws, base + r0 * rs + (comp * 2 + d) * W, [[rs, 128], [1, W]])
                    b.dma(dst(0), dec); b.dma(dst(1), kk); b.dma(dst(2), t2); b.dma(dst(3), t1); b.dma(dst(4), r_)
                    b.tt(t2, t1, r_, ALU.mult)
                    b.tt(t2, t2, rkr, ALU.mult)
                    b.reduce(sh, t2.rearrange("p (h k) -> p h k", h=H))
                    if d == 0:
                        b.tt(bon.rearrange("p (h k) -> p h k", h=H), v_.rearrange("p (h k) -> p h k", h=H), fview(sh, [[1, H], [0, 64]]), ALU.mult)
                    else:
                        b.tt(t2.rearrange("p (h k) -> p h k", h=H), v_.rearrange("p (h k) -> p h k", h=H), fview(sh, [[1, H], [0, 64]]), ALU.mult)
                        b.tt(bon, bon, t2, ALU.add)
                b.dma(g.rbon[bi, r0:r0 + 128, :], bon)
    b.pop()
    b.push()
    NP = NB * 64
    CH = 2 * H
    FS = CH * 64
    TB = 4
    VT = 128
    Ss = [b.sb("rs_S%d" % i, [NP, FS]) for i in range(2)]
    b.memset(Ss[0], 0.0); b.memset(Ss[1], 0.0)
    q3s = [b.sb("rs_q3%d" % i, [NP, FS]) for i in range(2)]
    yqs = [b.sb("rs_yq%d" % i, [NP, FS]) for i in range(2)]
    prev_r = None
    Bc = [b.sb("rs_Bc%d" % i, [NP, TB, 5, FS]) for i in range(3)]
    Vt = [b.sb("rs_Vt%d" % i, [NP, CH, VT]) for i in range(2)]
    Yb = [b.sb("rs_Yb%d" % i, [NP, CH, VT]) for i in range(2)]
    q1 = b.sb("rs_q1", [NP, FS]); q2 = b.sb("rs_q2", [NP, FS]); q3 = b.sb("rs_q3", [NP, FS]); q4 = b.sb("rs_q4", [NP, FS])
    sa = b.sb("rs_sa", [NP, CH])
    v3 = lambda a: a.rearrange("p (c k) -> p c k", c=CH)
    for tau in range(LT):
        blk, ti = divmod(tau, TB)
        if ti == 0:
            bc = Bc[blk % 3]
            tq = tau + TB - 1
            tok1 = (LC - 1 - tq) if tq < LC else (LC + (L - 1 - (tq - LC)))
            for bi in range(NB):
                b.dma(bc[bi * 64:(bi + 1) * 64, :, :, 0:W], mkap(g.rws, g.rws[bi].offset + tau * 5 * FS, [[0, 64], [5 * FS, TB], [FS, 5], [1, W]]), q=("sp" if bi == 0 else "act"))
                b.dma(bc[bi * 64:(bi + 1) * 64, :, :, W:FS], mkap(g.rws, g.rws[bi].offset + tok1 * 5 * FS + W, [[0, 64], [5 * FS, TB], [FS, 5], [1, W]]), q=("sp" if bi == 0 else "act"))
        vb, vi = divmod(tau, VT)
        if vi == 0:
            vt = Vt[vb % 2]; yb = Yb[vb % 2]
            for bi in range(NB):
                for h in range(H):
                    for d in range(2):
                        b.dma(vt[bi * 64:(bi + 1) * 64, d * H + h, :], g.vsd[bi, d, h * 64:(h + 1) * 64, tau:tau + VT])
        def comp(j):
            return mkap(bc, bc.offset + ti * 5 * FS + j * FS, [list(bc.ap[0]), [(TB - 1 - 2 * ti) * 5 * FS + W, 2], [64, H], [1, 64]])
        w_b, kk_b, kka_b, kd_b, r_b = [comp(j) for j in range(5)]
        v4 = lambda a: a.rearrange("p (d h k) -> p d h k", d=2, h=H)
        Sp, Sn = Ss[(tau + 1) % 2], Ss[tau % 2]
        q3_ = q3s[tau % 2]
        b.k.op("pool", lambda E: E.tensor_tensor(out=v4(q3_), in0=kd_b, in1=mkap(vt, vt.offset + vi, [list(vt.ap[0]), [H * VT, 2], [VT, H], [0, 64]]), op=ALU.mult), [key(bc), key(vt)], [key(q3_)])
        if prev_r is not None:
            yq_ = yqs[tau % 2]
            b.tt(v4(yq_), v4(Sp), prev_r[0], ALU.mult, e="pool")
        b.tt(v4(q1), v4(Sp), kk_b, ALU.mult)
        b.reduce(sa, v3(q1))
        b.tt(v4(q4), kka_b, mkap(sa, sa.offset, [list(sa.ap[0]), [H, 2], [1, H], [0, 64]]), ALU.mult)
        b.tt(q4, q4, q3_, ALU.subtract)
        b.tt(v4(q2), v4(Sp), w_b, ALU.mult)
        b.tt(Sn, q2, q4, ALU.subtract)
        if prev_r is not None:
            b.reduce(prev_r[1], v3(yq_))
        prev_r = (r_b, mkap(yb, yb.offset + vi, [list(yb.ap[0]), [VT, CH]]), yb)
        if tau == LT - 1 or vi == VT - 1:
            yq_ = yqs[(tau + 1) % 2]
            b.tt(v4(yq_), v4(Sn), prev_r[0], ALU.mult, e="pool")
            b.reduce(prev_r[1], v3(yq_))
            prev_r = None
        if vi == VT - 1 or tau == LT - 1:
            t0 = tau - vi
            for bi in range(NB):
                for h in range(H):
                    for d in range(2):
                        b.dma(g.ysd[bi, d, h * 64:(h + 1) * 64, t0:t0 + VT], yb[bi * 64:(bi + 1) * 64, d * H + h, :])
    b.pop()
    b.push()
    lng = brow("rw_lng2", g.w["rwkv_ln_g"], l * W, W)
    lnb = brow("rw_lnb2", g.w["rwkv_ln_b"], l * W, W)
    y0 = b.sb("ro_y0", [PW, 128]); y1 = b.sb("ro_y1", [PW, 128])
    yt = b.sb("ro_yt", [128, W]); t1 = b.sb("ro_t1", [128, W]); t2 = b.sb("ro_t2", [128, W])
    mu = b.sb("ro_mu", [128, H]); var = b.sb("ro_var", [128, H])
    h3 = lambda a: a.rearrange("p (h k) -> p h k", h=H)
    for bi in range(NB):
        for (s0, n) in segs:
            if s0 == 0 and l == c.depth - 1 and not getattr(g, "force_ctx", False):
                continue
            for t in range(n // 128):
                r0 = s0 + t * 128
                tlo = s0 + n - 128 - (r0 - s0)
                for pw in range(NPW):
                    b.dma(y0, g.ysd[bi, 0, pw * PW:(pw + 1) * PW, r0:r0 + 128])
                    b.dma(y1, g.ysd[bi, 1, pw * PW:(pw + 1) * PW, tlo:tlo + 128])
                    b.tt(y0, y0, rev(y1), ALU.add)
                    b.tr(g.pb[0][:, 0:PW], y0, g.ident[0:PW, 0:PW])
                    b.copy(yt[:, pw * PW:(pw + 1) * PW], g.pb[0][:, 0:PW], e="act")
                b.reduce(mu, h3(yt))
                b.ts(mu, mu, 1.0 / 64, None, op0=ALU.mult)
                b.tt(h3(t1), h3(yt), fview(mu, [[1, H], [0, 64]]), ALU.subtract)
                b.tt(t2, t1, t1, ALU.mult)
                b.reduce(var, h3(t2))
                b.ts(var, var, 1.0 / 64, 64e-5, op0=ALU.mult, op1=ALU.add)
                b.act(var, var, AF.Sqrt)
                b.recip(var, var)
                b.tt(h3(t1), h3(t1), fview(var, [[1, H], [0, 64]]), ALU.mult)
                b.tt(t1, t1, lng, ALU.mult)
                b.tt(t1, t1, lnb, ALU.add)
                b.dma(t2, g.rbon[bi, r0:r0 + 128, :])
                b.tt(t1, t1, t2, ALU.add)
                b.dma(t2, g.rg[bi, r0:r0 + 128, :])
                b.tt(t1, t1, t2, ALU.mult)
                b.dma(g.ymix[bi, r0:r0 + 128, W:2 * W], t1)
    b.pop()


def stage_peer(g, l):
    b, c = g.b, g.c
    D, KD, PH, NK, LT, LC, NB = c.D, c.KD, c.PH, c.NK, c.LT, c.LC, c.NB
    NJ = PH * 2
    need_ctx = (l < c.depth - 1) or getattr(g, "force_ctx", False)
    NHF = max(1, D // 512)
    HW_ = D // NHF
    NTB = 2
    b.push()
    uT_r_pre = g.w["peer_uT"][l].rearrange("(kd p) e -> p kd e", p=128)
    keysT = b.sb("pe_keysT", [128, NJ, NK])
    b.dma(keysT, g.w["peer_keysT"][l].rearrange("h c q k -> q (h c) k"))
    a2 = b.sb("pe_a2", [128, D]); s2 = b.sb("pe_s2", [128, D]); g5 = b.sb("pe_g5", [128, D])
    iot = b.sb("pe_iot", [128, 128])
    b.k.op("pool", lambda E: E.iota(iot, pattern=[[1, 128]], base=0, channel_multiplier=0, allow_small_or_imprecise_dtypes=True), [], [key(iot)])
    thr16 = b.sb("pe_thr16", [128, 16])
    b.ts(thr16, iot[:, 0:16], 16.0, 16.0, op0=ALU.mult, op1=ALU.add)
    xk = [b.sb("pe_xk%d" % i, [128, D]) for i in range(NTB)]
    junk = b.sb("pe_junk", [128, D])
    h2 = junk
    ss = b.sb("pe_ss", [128, 1])
    h2T = b.sb("pe_h2T", [128, KD, NTB * 128])
    wqc = [b.sb("pe_wqc%d" % i, [128, KD, 128]) for i in range(2)]
    qTs = b.sb("pe_qTs", [128, NTB * 128])
    scs = [b.sb("pe_sc%d" % i, [128, NJ, NK]) for i in range(NTB)]
    scw = b.sb("pe_scw", [128, 256])
    sv = b.sb("pe_sv", [128, NJ, 16])
    si = b.sb("pe_si", [128, NJ, 16], U32)
    sif = b.sb("pe_sif", [128, NJ, 16])
    cand = b.sb("pe_cand", [128, PH, 256])
    tsv = b.sb("pe_ts", [128, PH, 16])
    pos = b.sb("pe_pos", [128, PH, 16], U32)
    posf = b.sb("pe_posf", [128, PH, 16])
    aq = b.sb("pe_aq", [128, PH, 16]); bq = b.sb("pe_bq", [128, PH, 16])
    oh = b.sb("pe_oh", [128, PH, 16, 16])
    ik = b.sb("pe_ik", [128, PH * 16]); jk = b.sb("pe_jk", [128, PH * 16]); val = b.sb("pe_val", [128, PH * 16])
    zz = b.sb("pe_zz", [128, PH])
    KK = PH * 16
    ikT = b.sb("pe_ikT", [KK, 128]); jkT = b.sb("pe_jkT", [KK, 128]); valT = b.sb("pe_valT", [KK, 128])
    A = b.sb("pe_A", [KK, 64, NK], BF16)
    Bv = b.sb("pe_Bv", [KK, 64, NK], BF16)
    Wsb = b.sb("pe_Wsb", [128, NTB, 128, NK], BF16)
    UW = KD * 128
    uvc = [b.sb("pe_uvc%d" % i, [128, UW + D], BF16) for i in range(3)]
    uc = [t[:, 0:UW].rearrange("p (k e) -> p k e", k=KD) for t in uvc]
    vc = [t[:, UW:UW + D] for t in uvc]
    sgs = [b.sb("pe_sg%d" % i, [128, NTB * 128], BF16) for i in range(2)]
    G = [b.sb("pe_G%d" % i, [128, NTB * 128], BF16) for i in range(2)]
    h2Tb = b.sb("pe_h2Tb", [128, KD, NTB * 128], BF16)
    for i in range(NK):
        uf = wqc[i % 2]
        b.dma(uf, uT_r_pre[:, :, i * 128:(i + 1) * 128])
        ub = uc[i % 3]
        b.copy(ub, uf, e=("dve" if i % 2 else "pool"))
        b.dma(g.puv16[i, :, 0:UW], uvc[i % 3][:, 0:UW], q="act")
        vf = xk[i % 2]
        b.dma(vf, g.w["peer_v"][l, i * 128:(i + 1) * 128, :])
        vb = vc[i % 3]
        b.copy(vb, vf, e="act")
        b.dma(g.puv16[i, :, UW:UW + D], vb, q="act")
    wq_r = g.w["peer_wq"][l].rearrange("(kd p) c -> p kd c", p=128)
    uT_r = g.w["peer_uT"][l].rearrange("(kd p) e -> p kd e", p=128)
    mk_ = getattr(g, "mark", lambda n: None)
    mk_("pe:conv_done")
    for bi in range(NB):
        segs = [(2, 0, LC), (bi, LC, c.L)] if need_ctx else [(bi, LC, c.L)]
        for (r, s0, n) in segs:
            b.dma(a2, mkap(g.modv, (r * 6 + 4) * D, [[0, 128], [1, D]]))
            b.dma(s2, mkap(g.modv, (r * 6 + 3) * D, [[0, 128], [1, D]]))
            b.dma(g5, mkap(g.modv, (r * 6 + 5) * D, [[0, 128], [1, D]]))
            tiles = list(range(n // 128))
            for b0 in range(0, len(tiles), NTB):
                blk = tiles[b0:b0 + NTB]
                ntok = len(blk) * 128
                for ti, t in enumerate(blk):
                    mk_("pe:tile_start")
                    r0 = s0 + t * 128
                    x_ = xk[ti]
                    b.dma(x_, g.xres[bi, r0:r0 + 128, :])
                    b.act(junk, x_, AF.Square, accum_out=ss)
                    b.ts(ss, ss, 1.0 / D, EPS, op0=ALU.mult, op1=ALU.add)
                    b.act(ss, ss, AF.Sqrt)
                    b.recip(ss, ss)
                    b.stt(junk, x_, ss, a2, ALU.mult, ALU.mult)
                    b.tt(h2, junk, s2, ALU.add)
                    for kd in range(KD):
                        pp = g.pb[kd % 2]
                        b.tr(pp[:, 0:128], h2[:, kd * 128:(kd + 1) * 128], g.ident)
                        b.copy(h2T[:, kd, ti * 128:(ti + 1) * 128], pp[:, 0:128], e=("act" if kd % 2 else "dve"))
                for j in range(NJ):
                    wq_ = wqc[j % 2]
                    b.dma(wq_, wq_r[:, :, j * 128:(j + 1) * 128], q=("sp" if j % 2 == 0 else "act"))
                    pq = g.pb[2]
                    for kd in range(KD):
                        b.mm(pq[:, 0:ntok], wq_[:, kd, :], h2T[:, kd, 0:ntok], start=(kd == 0), stop=(kd == KD - 1))
                    b.copy(qTs[:, 0:ntok], pq[:, 0:ntok], e="act")
                    for ti in range(len(blk)):
                        b.mm(g.pb[3][:, ti * NK:(ti + 1) * NK], qTs[:, ti * 128:(ti + 1) * 128], keysT[:, j, :])
                        b.copy(scs[ti][:, j, :], g.pb[3][:, ti * NK:(ti + 1) * NK])
                for ti, t in enumerate(blk):
                    sc = scs[ti]
                    mk_("pe:scores_done")
                    for j in range(NJ):
                        b.k.op("dve", lambda E: E.max(out=sv[:, j, 0:8], in_=sc[:, j, :]), [key(sc)], [key(sv)])
                        b.k.op("dve", lambda E: E.max_index(out=si[:, j, 0:8], in_max=sv[:, j, 0:8], in_values=sc[:, j, :]), [key(sc), key(sv)], [key(si)])
                        b.k.op("dve", lambda E: E.match_replace(out=scw[:, 0:NK], in_to_replace=sv[:, j, 0:8], in_values=sc[:, j, :], imm_value=-1e30), [key(sc), key(sv)], [key(scw)])
                        b.k.op("dve", lambda E: E.max(out=sv[:, j, 8:16], in_=scw[:, 0:NK]), [key(scw)], [key(sv)])
                        b.k.op("dve", lambda E: E.max_index(out=si[:, j, 8:16], in_max=sv[:, j, 8:16], in_values=scw[:, 0:NK]), [key(scw), key(sv)], [key(si)])
                    b.copy(sif, si)
                    svf = sv.rearrange("p j r -> p (j r)")
                    b.tt(cand.rearrange("p h (a b) -> p h a b", a=16), fview(svf, [[32, PH], [1, 16], [0, 16]]), fview(svf, [[32, PH], [0, 16], [1, 16]], off=16), ALU.add)
                    for h in range(PH):
                        b.k.op("dve", lambda E: E.max(out=tsv[:, h, 0:8], in_=cand[:, h, :]), [key(cand)], [key(tsv)])
                        b.k.op("dve", lambda E: E.max_index(out=pos[:, h, 0:8], in_max=tsv[:, h, 0:8], in_values=cand[:, h, :]), [key(cand), key(tsv)], [key(pos)])
                        b.k.op("dve", lambda E: E.match_replace(out=scw, in_to_replace=tsv[:, h, 0:8], in_values=cand[:, h, :], imm_value=-1e30), [key(cand), key(tsv)], [key(scw)])
                        b.k.op("dve", lambda E: E.max(out=tsv[:, h, 8:16], in_=scw), [key(scw)], [key(tsv)])
                        b.k.op("dve", lambda E: E.max_index(out=pos[:, h, 8:16], in_max=tsv[:, h, 8:16], in_values=scw), [key(scw), key(tsv)], [key(pos)])
                    b.copy(posf, pos)
                    posff = posf.rearrange("p h r -> p (h r)")
                    b.tt(oh, fview(posff, [[16, PH], [1, 16], [0, 16]]), fview(thr16, [[0, PH], [0, 16], [1, 16]]), ALU.is_ge)
                    b.reduce(aq.rearrange("p h r -> p (h r)"), oh.rearrange("p h k a -> p (h k) a"))
                    b.stt(bq, aq, -16.0, posf, ALU.mult, ALU.add)
                    siff = sif.rearrange("p j r -> p (j r)")
                    for (src, cidx, dst) in ((aq, 0, ik), (bq, 1, jk)):
                        srcf = src.rearrange("p h r -> p (h r)")
                        b.tt(oh, fview(srcf, [[16, PH], [1, 16], [0, 16]]), fview(iot, [[0, PH], [0, 16], [1, 16]]), ALU.is_equal)
                        b.tt(oh, oh, fview(siff, [[32, PH], [0, 16], [1, 16]], off=cidx * 16), ALU.mult)
                        b.reduce(dst, oh.rearrange("p h k a -> p (h k) a"))
                    tsf = tsv.rearrange("p h r -> p (h r)")
                    b.tt(val.rearrange("p (h r) -> p h r", h=PH), tsv, fview(tsf, [[16, PH], [0, 16]]), ALU.subtract)
                    b.act(val, val, AF.Exp)
                    b.reduce(zz, val.rearrange("p (h r) -> p h r", h=PH))
                    b.recip(zz, zz)
                    b.tt(val.rearrange("p (h r) -> p h r", h=PH), val.rearrange("p (h r) -> p h r", h=PH), fview(zz, [[1, PH], [0, 16]]), ALU.mult)
                    for (src, dstT) in ((ik, ikT), (jk, jkT), (val, valT)):
                        b.tr(g.pb[2][0:KK, 0:128], src, g.ident)
                        b.copy(dstT, g.pb[2][0:KK, 0:128], e="act")
                    mk_("pe:route_done")
                    for hh in range(2):
                        b.tt(A, fview(ikT, [[1, 64], [0, NK]], off=hh * 64), fview(iot[0:KK, :], [[0, 64], [1, NK]]), ALU.is_equal)
                        b.tt(Bv, fview(jkT, [[1, 64], [0, NK]], off=hh * 64), fview(iot[0:KK, :], [[0, 64], [1, NK]]), ALU.is_equal)
                        b.tt(Bv, Bv, fview(valT, [[1, 64], [0, NK]], off=hh * 64), ALU.mult, e="pool")
                        for tk in range(64):
                            tkk = hh * 64 + tk
                            pw = g.pb[4 + (tk // 4) % 2]
                            b.mm(pw[:, (tk % 4) * 128:(tk % 4 + 1) * 128], Bv[:, tk, :], A[:, tk, :])
                            if tk % 4 == 3:
                                b.copy(Wsb[:, ti, tkk - 3:tkk + 1, :], pw.rearrange("p (t i) -> p t i", t=4), e=("act" if (tk // 4) % 2 else "dve"))
                mk_("pe:wbuild_done")
                po = [[g.pb[ti * NHF + hf] for hf in range(NHF)] for ti in range(len(blk))]
                b.copy(h2Tb[:, :, 0:ntok], h2T[:, :, 0:ntok])
                NBUF = len(uc)

                def load_tabs(i):
                    b.dma(uvc[i % NBUF], g.puv16[i], q=("sp" if i % 2 == 0 else "act"))

                def act_mm(i):
                    pa = g.pb[4 + i % 2]
                    for kd in range(KD):
                        b.mm(pa[:, 0:ntok], uc[i % NBUF][:, kd, :], h2Tb[:, kd, 0:ntok], start=(kd == 0), stop=(kd == KD - 1))

                for i in range(min(NBUF - 1, NK)):
                    load_tabs(i)
                act_mm(0)
                for i in range(NK):
                    if i + NBUF - 1 < NK:
                        load_tabs(i + NBUF - 1)
                    if i + 1 < NK:
                        act_mm(i + 1)
                    pa = g.pb[4 + i % 2]
                    sg = sgs[i % 2]
                    G_ = G[i % 2]
                    b.act(sg[:, 0:ntok], pa[:, 0:ntok], AF.Gelu_apprx_tanh)
                    b.tt(G_[:, 0:ntok].rearrange("p (t k) -> p t k", k=128), sg[:, 0:ntok].rearrange("p (t k) -> p t k", k=128),
                         mkap(Wsb, Wsb.offset + i, [list(Wsb.ap[0]), [128 * NK, len(blk)], [NK, 128]]), ALU.mult)
                    v_ = vc[i % NBUF]
                    for ti in range(len(blk)):
                        for hf in range(NHF):
                            b.mm(po[ti][hf][:, 0:HW_], G_[:, ti * 128:(ti + 1) * 128], v_[:, hf * HW_:(hf + 1) * HW_], start=(i == 0), stop=(i == NK - 1))
                mk_("pe:chunks_done")
                for ti, t in enumerate(blk):
                    r0 = s0 + t * 128
                    for hf in range(NHF):
                        b.tt(junk[:, hf * HW_:(hf + 1) * HW_], po[ti][hf][:, 0:HW_], g5[:, hf * HW_:(hf + 1) * HW_], ALU.mult)
                    b.tt(xk[ti], xk[ti], junk, ALU.add)
                    b.dma(g.xres[bi, r0:r0 + 128, :], xk[ti])
    b.pop()


_CACHE = {}


def build_program(cfg):
    g = setup(cfg)
    b = g.b
    for l in range(cfg.depth):
        for s in (stage_mod, stage_in, stage_lru, stage_rwkv, stage_mlstm, stage_hyena, stage_merge):
            b.push()
            s(g, l)
            b.pop()
        stage_peer(g, l)
    b.push()
    stage_final(g)
    b.pop()
    b.k.finish()
    return g


def kernel(**inputs):
    cfg = Cfg()
    inp = {k: np.ascontiguousarray(np.asarray(v, dtype=np.float32)) for k, v in inputs.items()}
    if "g" not in _CACHE:
        _CACHE["g"] = build_program(cfg)
    g = _CACHE["g"]
    shared = {n: inp[n] for n in WNAMES}
    shared.update(host_layout(inp))
    shared.update(hy_consts(cfg))
    n_cores = 8
    in_maps = []
    for i in range(n_cores):
        rows = np.stack([inp["c"][2 * i], inp["c"][2 * i + 1], inp["c_ctx"]], 0)
        c3T = np.ascontiguousarray(rows.T.reshape(cfg.KD, 128, 3).transpose(1, 0, 2))
        m = dict(shared)
        m["x"] = np.ascontiguousarray(inp["x"][2 * i:2 * i + 2])
        m["ctx"] = np.ascontiguousarray(inp["ctx"][2 * i:2 * i + 2])
        m["c3T"] = c3T
        in_maps.append(m)
    res = run_bass_kernel_spmd(g.nc, in_maps, core_ids=list(range(n_cores)))
    return np.concatenate([np.asarray(r["out"], dtype=np.float32) for r in res.results], axis=0)
```

```python
from concourse.bass_utils import run_bass_kernel_spmd
import numpy as np
import concourse.bass as bass
import concourse.mybir as mybir

F32 = mybir.dt.float32
BF16 = mybir.dt.bfloat16
I32 = mybir.dt.int32
U32 = mybir.dt.uint32
AF = mybir.ActivationFunctionType
ALU = mybir.AluOpType
AX = mybir.AxisListType
NDS = 12
EPS = 1e-6


class Cfg:
    def __init__(s, D=1024, L=2048, LC=256, GW=64, NB=2, PH=8, NK=128, depth=2):
        s.D, s.L, s.LC, s.GW, s.NB, s.PH, s.NK, s.depth = D, L, LC, GW, NB, PH, NK, depth
        s.KD = D // 128
        s.LT = L + LC
        s.W = D // 4
        s.H = s.W // 64
        W, H = s.W, s.H
        s.splits = (W, W, W, W, W, 32, 32, 64, W, W, W, W, 4 * H, W, W, W)
        s.DIN = sum(s.splits)
        s.off = np.concatenate([[0], np.cumsum(s.splits)]).tolist()
        s.DQ = 2 * 128
        s.NE = NK * NK


class KB:
    def __init__(self, nc):
        self.nc = nc
        self.eng = {"pe": nc.tensor, "dve": nc.vector, "act": nc.scalar, "pool": nc.gpsimd, "sp": nc.sync}
        self.sem, self.cnt = {}, {}
        for e in self.eng:
            self.sem[e] = nc.semaphore("s_" + e).__enter__()
            self.cnt[e] = 0
        self.dq = {}
        for q in ("sp", "act", "pool"):
            self.dq[q] = [q, [nc.semaphore("d_%s%d" % (q, i)).__enter__() for i in range(NDS)], 0]
        self.lastw, self.readers, self.waited = {}, {}, {}
        self.n_instr = 0

    def _wait(self, e, tk, val):
        if tk[0] == "e":
            key = (e,) + tk
            if self.waited.get(key, 0) >= val:
                return
            self.waited[key] = val
            self.eng[e].wait_ge(self.sem[tk[1]], val)
        else:
            v = 16 * (val // NDS + 1)
            key = (e,) + tk
            if self.waited.get(key, 0) >= v:
                return
            self.waited[key] = v
            self.eng[e].wait_ge(self.dq[tk[1]][1][tk[2]], v)
        self.n_instr += 1

    def _deps(self, e, reads, writes):
        need = {}
        for r in reads:
            lw = self.lastw.get(r)
            if lw is not None:
                need[lw[0]] = max(need.get(lw[0], -1), lw[1])
        for w in writes:
            lw = self.lastw.get(w)
            if lw is not None:
                need[lw[0]] = max(need.get(lw[0], -1), lw[1])
            for tk, v in self.readers.get(w, {}).items():
                need[tk] = max(need.get(tk, -1), v)
        for tk, v in need.items():
            self._wait(e, tk, v)

    def _commit(self, tk, val, reads, writes):
        for r in reads:
            d = self.readers.setdefault(r, {})
            d[tk] = max(d.get(tk, -1), val)
        for w in writes:
            self.lastw[w] = (tk, val)
            self.readers[w] = {}

    def op(self, e, fn, reads=(), writes=()):
        self._deps(e, reads, writes)
        ins = fn(self.eng[e])
        self.cnt[e] += 1
        ins.then_inc(self.sem[e], 1)
        self._commit(("e", e), self.cnt[e], reads, writes)
        self.n_instr += 1
        return ins

    def dma(self, q, out, in_, reads=(), writes=(), **kw):
        e, sems, i = self.dq[q]
        self._deps(e, reads, writes)
        if i >= NDS:
            self._wait(e, ("d", q, i % NDS), i - NDS)
        ins = self.eng[e].dma_start(out=out, in_=in_, **kw)
        ins.then_inc(sems[i % NDS], 16)
        self.dq[q][2] = i + 1
        self._commit(("d", q, i % NDS), i, reads, writes)
        self.n_instr += 1
        return ins

    def finish(self):
        for f in self.eng:
            if self.cnt[f] > 0:
                self._wait("sp", ("e", f), self.cnt[f])
        for q in self.dq:
            n = self.dq[q][2]
            for i in range(max(0, n - NDS), n):
                self._wait("sp", ("d", q, i % NDS), i)


def key(ap):
    return ap.tensor.name


class B:
    def __init__(self, nc, cfg):
        self.nc, self.cfg = nc, cfg
        self.k = KB(nc)
        self.dmaq = 0
        self.scopes = [[]]

    def sb(self, name, shape, dt=F32):
        self.uid = getattr(self, "uid", 0) + 1
        name = "%s_u%d" % (name, self.uid)
        gd = self.nc.sbuf_tensor(name, list(shape), dt)
        t = gd.__enter__()
        self.scopes[-1].append(gd)
        return t.ap() if hasattr(t, "ap") and callable(t.ap) else t

    def push(self):
        self.scopes.append([])

    def pop(self):
        self.barrier()
        for gd in reversed(self.scopes.pop()):
            gd.__exit__(None, None, None)

    def barrier(self):
        k = self.k
        for e in k.eng:
            for f in k.eng:
                if f != e and k.cnt[f] > 0:
                    k._wait(e, ("e", f), k.cnt[f])
            for q in k.dq:
                n = k.dq[q][2]
                for i in range(max(0, n - NDS), n):
                    k._wait(e, ("d", q, i % NDS), i)

    def ps(self, name, shape, dt=F32):
        return self.nc.alloc_psum_tensor(name, list(shape), dt).ap()

    def dram(self, name, shape, dt=F32, kind="Internal"):
        return self.nc.dram_tensor(name, list(shape), dt, kind=kind).ap()

    def _rw(self, outs, ins):
        return [key(a) for a in ins if hasattr(a, "tensor")], [key(a) for a in outs]

    def dma(self, out, in_, q=None, **kw):
        if q is None:
            q = "sp"
        r, w = self._rw([out], [in_])
        return self.k.dma(q, out, in_, reads=r, writes=w, **kw)

    def act(self, out, in_, func, bias=0.0, scale=1.0, accum_out=None, e="act"):
        r, w = self._rw([out] + ([accum_out] if accum_out is not None else []), [in_, bias, scale])
        kw = {}
        if accum_out is not None:
            kw["accum_out"] = accum_out
        return self.k.op(e, lambda E: E.activation(out=out, in_=in_, func=func, bias=bias, scale=scale, **kw), r, w)

    def tt(self, out, in0, in1, op, e="dve"):
        r, w = self._rw([out], [in0, in1])
        return self.k.op(e, lambda E: E.tensor_tensor(out=out, in0=in0, in1=in1, op=op), r, w)

    def ts(self, out, in0, s1, s2=None, op0=ALU.mult, op1=None, e="dve", accum_out=None):
        r, w = self._rw([out] + ([accum_out] if accum_out is not None else []), [in0, s1, s2])
        kw = {}
        if op1 is not None:
            kw["op1"] = op1
        if accum_out is not None:
            kw["accum_out"] = accum_out
        return self.k.op(e, lambda E: E.tensor_scalar(out=out, in0=in0, scalar1=s1, scalar2=s2, op0=op0, **kw), r, w)

    def stt(self, out, in0, scalar, in1, op0, op1, e="dve"):
        r, w = self._rw([out], [in0, scalar, in1])
        return self.k.op(e, lambda E: E.scalar_tensor_tensor(out=out, in0=in0, scalar=scalar, in1=in1, op0=op0, op1=op1), r, w)

    def copy(self, out, in_, e="dve"):
        r, w = self._rw([out], [in_])
        if e == "act":
            return self.k.op(e, lambda E: E.copy(out=out, in_=in_), r, w)
        return self.k.op(e, lambda E: E.tensor_copy(out=out, in_=in_), r, w)

    def memset(self, out, val, e="dve"):
        r, w = self._rw([out], [])
        return self.k.op(e, lambda E: E.memset(out, val), r, w)

    def reduce(self, out, in_, op=ALU.add, axis=AX.X, e="dve"):
        r, w = self._rw([out], [in_])
        return self.k.op(e, lambda E: E.tensor_reduce(out=out, in_=in_, axis=axis, op=op), r, w)

    def scan(self, out, d0, d1, init, op0=ALU.mult, op1=ALU.add, e="dve"):
        r, w = self._rw([out], [d0, d1, init])
        return self.k.op(e, lambda E: E.tensor_tensor_scan(out=out, data0=d0, data1=d1, initial=init, op0=op0, op1=op1), r, w)

    def recip(self, out, in_):
        r, w = self._rw([out], [in_])
        return self.k.op("dve", lambda E: E.reciprocal(out=out, in_=in_), r, w)

    def mm(self, out, lhsT, rhs, start=True, stop=True):
        r, w = self._rw([out], [lhsT, rhs])
        if not start:
            r = r + w
        return self.k.op("pe", lambda E: E.matmul(out, lhsT, rhs, start=start, stop=stop), r, w)

    def tr(self, out, in_, ident):
        r, w = self._rw([out], [in_, ident])
        return self.k.op("pe", lambda E: E.transpose(out=out, in_=in_, identity=ident), r, w)


def mkap(t, offset, pairs):
    return bass.AP(t.tensor, offset, [list(p) for p in pairs])


def fview(t, pairs, off=0):
    return bass.AP(t.tensor, t.offset + off, [list(t.ap[0])] + [list(p) for p in pairs])


def rev(t):
    n = t.shape[-1]
    st = t.ap[-1][0]
    return bass.AP(t.tensor, t.offset + (n - 1) * st, [list(p) for p in t.ap[:-1]] + [[-st, n]])


WNAMES = ["ada_w", "ada_b", "norm1_g", "norm2_g", "w_in", "w_out", "grp_g",
          "lru_conv_w", "lru_conv_b", "lru_wr", "lru_br", "lru_wi", "lru_bi", "lru_lam",
          "rwkv_mu", "rwkv_w0", "rwkv_w2", "rwkv_a0", "rwkv_a2", "rwkv_g2", "rwkv_kk", "rwkv_ka", "rwkv_rk",
          "rwkv_ln_g", "rwkv_ln_b", "mlstm_conv_w", "mlstm_conv_b", "mlstm_gate_b",
          "hy_conv_w", "hy_conv_b", "hy_w1", "hy_b1", "hy_w2", "hy_b2", "hy_w3", "hy_freq", "hy_bias",
          "peer_wq", "peer_v", "final_g"]


def host_layout(inp):
    return {"peer_keysT": np.ascontiguousarray(np.transpose(inp["peer_keys"], (0, 1, 2, 4, 3))),
            "peer_uT": np.ascontiguousarray(np.transpose(inp["peer_u"], (0, 2, 1)))}


def wshapes(c):
    Dp, W, H = c.depth, c.W, c.H
    return dict(
        ada_w=(Dp, c.D, 6 * c.D), ada_b=(Dp, 6 * c.D), norm1_g=(Dp, c.D), norm2_g=(Dp, c.D), w_in=(Dp, c.D, c.DIN),
        w_out=(Dp, c.D, c.D), grp_g=(Dp, c.D), lru_conv_w=(Dp, 4, W), lru_conv_b=(Dp, W), lru_wr=(Dp, 2, H, 64, 64),
        lru_br=(Dp, 2, W), lru_wi=(Dp, 2, H, 64, 64), lru_bi=(Dp, 2, W), lru_lam=(Dp, 2, W),
        rwkv_mu=(Dp, 3, 2, W), rwkv_w0=(Dp, 2, W), rwkv_w2=(Dp, 2, 32, W), rwkv_a0=(Dp, 2, W), rwkv_a2=(Dp, 2, 32, W),
        rwkv_g2=(Dp, 64, W), rwkv_kk=(Dp, W), rwkv_ka=(Dp, W), rwkv_rk=(Dp, H, 64), rwkv_ln_g=(Dp, W), rwkv_ln_b=(Dp, W),
        mlstm_conv_w=(Dp, 4, 2 * W), mlstm_conv_b=(Dp, 2 * W), mlstm_gate_b=(Dp, 2, 2, H),
        hy_conv_w=(Dp, 3, 3 * W), hy_conv_b=(Dp, 3 * W), hy_w1=(Dp, 33, 64), hy_b1=(Dp, 64), hy_w2=(Dp, 64, 64),
        hy_b2=(Dp, 64), hy_w3=(Dp, 64, 4 * W), hy_freq=(Dp, 2, 64), hy_bias=(Dp, 2, W),
        peer_wq=(Dp, c.D, c.PH * c.DQ), peer_v=(Dp, c.NE, c.D),
        final_g=(c.D,))


class Ctx:
    pass


def setup(cfg, stop_after=None):
    nc = bass.Bass("TRN2", target_bir_lowering=False)
    b = B(nc, cfg)
    c = cfg
    g = Ctx()
    g.b, g.c, g.nc = b, c, nc
    g.x = b.dram("x", [c.NB, c.L, c.D], kind="ExternalInput")
    g.ctx = b.dram("ctx", [c.NB, c.LC, c.D], kind="ExternalInput")
    g.c3T = b.dram("c3T", [128, c.KD, 3], kind="ExternalInput")
    g.w = {}
    for n, s in wshapes(c).items():
        g.w[n] = b.dram(n, list(s), kind="ExternalInput")
    g.w["peer_keysT"] = b.dram("peer_keysT", [c.depth, c.PH, 2, 128, c.NK], kind="ExternalInput")
    g.w["peer_uT"] = b.dram("peer_uT", [c.depth, c.D, c.NE], kind="ExternalInput")
    g.out = b.dram("out", [c.NB, c.L, c.D], kind="ExternalOutput")
    g.modv = b.dram("modv", [3, 6, c.D])
    g.zT = b.dram("zT", [c.NB, c.DIN, c.LT])
    g.zt = b.dram("zt", [c.NB, c.LT, c.DIN])
    g.xres = b.dram("xres", [c.NB, c.LT, c.D])
    g.ymix = b.dram("ymix", [c.NB, c.LT, c.D])
    g.mlsc = b.dram("mlsc", [2, c.H, c.LT])
    g.mlsc2 = b.dram("mlsc2", [2, 2, c.H, c.LT])
    g.ident = b.sb("ident", [128, 128])
    g.identb = b.sb("identb", [128, 128], BF16)
    io = b.sb("iota_t", [128, 128])
    b.k.op("pool", lambda E: E.iota(io, pattern=[[1, 128]], base=0, channel_multiplier=-1, allow_small_or_imprecise_dtypes=True), [], [key(io)])
    b.ts(g.ident, io, 0.0, None, op0=ALU.is_equal)
    b.copy(g.identb, g.ident)
    g.negpi = b.sb("negpi", [128, 1])
    b.memset(g.negpi, -float(np.pi))
    hy_decl(g)
    rw_decl(g)
    g.neghalf = b.sb("neghalf", [128, 1])
    b.memset(g.neghalf, -0.5)
    g.pb = [b.ps("pb%d" % i, [128, 512]) for i in range(6)]
    g.pbb = [b.ps("pbb%d" % i, [128, 1024], BF16) for i in range(2)]
    return g


def stage_mod(g, l):
    b, c = g.b, g.c
    D, KD = c.D, c.KD
    c3 = b.sb("c3_%d" % l, [128, KD, 3])
    b.dma(c3, g.c3T)
    b.act(c3, c3, AF.Silu)
    modrow = b.sb("modrow%d" % l, [3, 6 * D])
    adab = b.sb("adab%d" % l, [3, 6 * D])
    b.dma(adab, g.w["ada_b"][l:l + 1, :].partition_broadcast(3) if False else mkap(g.w["ada_b"], l * 6 * D, [[0, 3], [1, 6 * D]]))
    aw = g.w["ada_w"][l].rearrange("(kd p) c -> p kd c", p=128)
    wts = [b.sb("adaw%d_%d" % (l, i), [128, KD, 512]) for i in range(2)]
    for cc in range(6 * D // 512):
        wt = wts[cc % 2]
        b.dma(wt, aw[:, :, cc * 512:(cc + 1) * 512])
        pp = g.pb[cc % 2]
        for kd in range(KD):
            b.mm(pp[0:3, :], c3[:, kd, :], wt[:, kd, :], start=(kd == 0), stop=(kd == KD - 1))
        b.tt(modrow[:, cc * 512:(cc + 1) * 512], pp[0:3, :], adab[:, cc * 512:(cc + 1) * 512], ALU.add)
    for slot, gn in ((1, "norm1_g"), (4, "norm2_g")):
        gr = b.sb("gr%d_%d" % (l, slot), [3, D])
        b.dma(gr, mkap(g.w[gn], l * D, [[0, 3], [1, D]]))
        sl = modrow[:, slot * D:(slot + 1) * D]
        b.stt(sl, sl, 1.0, gr, ALU.add, ALU.mult)
    b.dma(g.modv.rearrange("r s d -> r (s d)"), modrow)


def bcast_row(g, name, src_ap_dram, off, n, parts=128, t=None):
    if t is None:
        t = g.b.sb(name, [parts, n])
    g.b.dma(t, mkap(src_ap_dram, off, [[0, parts], [1, n]]))
    return t


def stage_in(g, l):
    b, c = g.b, g.c
    D, KD, DIN = c.D, c.KD, c.DIN
    wbf = b.sb("wbf", [128, KD, DIN], BF16)
    wst = [b.sb("wst%d" % i, [128, DIN]) for i in range(2)]
    for kd in range(KD):
        b.dma(wst[kd % 2], g.w["w_in"][l, kd * 128:(kd + 1) * 128, :])
        b.copy(wbf[:, kd, :], wst[kd % 2], e=("dve" if kd % 2 else "pool"))
    hT = b.sb("hT", [128, KD, c.L], BF16)
    xt = [b.sb("xt%d" % i, [128, D]) for i in range(2)]
    hb = [b.sb("hb%d" % i, [128, D], BF16) for i in range(2)]
    junk = b.sb("junk", [128, D])
    ss = b.sb("ss", [128, 1])
    stg = [b.sb("stg%d" % i, [128, 512]) for i in range(2)]
    stg2 = [b.sb("stg2_%d" % i, [128, DIN]) for i in range(2)]
    nst = 0
    a1 = b.sb("a1row", [128, D])
    s1 = b.sb("s1row", [128, D])
    for bi in range(c.NB):
        for seg in range(2):
            r = 2 if seg == 0 else bi
            Lu = c.LC if seg == 0 else c.L
            col0 = 0 if seg == 0 else c.LC
            if l == 0:
                src = g.ctx[bi] if seg == 0 else g.x[bi]
            else:
                src = g.xres[bi, col0:col0 + Lu, :]
            bcast_row(g, "a1row", g.modv, (r * 6 + 1) * D, D, t=a1)
            bcast_row(g, "s1row", g.modv, (r * 6 + 0) * D, D, t=s1)
            for t in range(Lu // 128):
                x_ = xt[t % 2]
                b.dma(x_, src[t * 128:(t + 1) * 128, :])
                b.act(junk, x_, AF.Square, accum_out=ss)
                b.ts(ss, ss, 1.0 / D, EPS, op0=ALU.mult, op1=ALU.add)
                b.act(ss, ss, AF.Sqrt)
                b.recip(ss, ss)
                b.stt(junk, x_, ss, a1, ALU.mult, ALU.mult)
                h_ = hb[t % 2]
                b.tt(h_, junk, s1, ALU.add)
                pt = g.pbb[t % 2]
                for kd in range(KD):
                    b.tr(pt[:, kd * 128:(kd + 1) * 128], h_[:, kd * 128:(kd + 1) * 128], g.identb)
                b.copy(hT[:, :, t * 128:(t + 1) * 128], pt[:, 0:KD * 128].rearrange("p (k t) -> p k t", k=KD), e="act")
            NBLK = min(512, Lu)
            fm_ranges = [(c.off[0], c.off[2]), (c.off[5], c.off[10]), (c.off[12], c.off[13])]
            fm_chunks = [(m0, min(128, r1 - m0)) for (r0_, r1) in fm_ranges for m0 in range(r0_, r1, 128)]
            for (m0, mw) in fm_chunks:
                for nb in range(Lu // NBLK):
                    pp = g.pb[nst % 2]
                    for kd in range(KD):
                        b.mm(pp[0:mw, 0:NBLK], wbf[:, kd, m0:m0 + mw], hT[:, kd, nb * NBLK:(nb + 1) * NBLK], start=(kd == 0), stop=(kd == KD - 1))
                    s_ = stg[nst % 2]
                    b.copy(s_[0:mw, 0:NBLK], pp[0:mw, 0:NBLK], e=("act" if nst % 2 else "dve"))
                    b.dma(g.zT[bi, m0:m0 + mw, col0 + nb * NBLK: col0 + (nb + 1) * NBLK], s_[0:mw, 0:NBLK])
                    nst += 1
            for t in range(Lu // 128):
                s2 = stg2[t % 2]
                tm_ranges = [(c.off[2], c.off[5]), (c.off[10], c.off[12]), (c.off[13], DIN)]
                tm_chunks = [(c0, min(512, r1 - c0)) for (r0_, r1) in tm_ranges for c0 in range(r0_, r1, 512)]
                for (c0, cw) in tm_chunks:
                    pp = g.pb[2 + nst % 2]
                    for kd in range(KD):
                        b.mm(pp[:, 0:cw], hT[:, kd, t * 128:(t + 1) * 128], wbf[:, kd, c0:c0 + cw], start=(kd == 0), stop=(kd == KD - 1))
                    b.copy(s2[:, c0:c0 + cw], pp[:, 0:cw], e=("act" if nst % 2 else "dve"))
                    nst += 1
                for (r0_, r1) in tm_ranges:
                    b.dma(g.zt[bi, col0 + t * 128: col0 + (t + 1) * 128, r0_:r1], s2[:, r0_:r1])


def colparam(g, t, j, src, off, n):
    g.b.dma(t[0:n, j:j + 1], mkap(src, off, [[1, n], [1, 1]]))


def nblocks(n, bs=512):
    out, s = [], 0
    while s < n:
        out.append((s, min(bs, n - s)))
        s += bs
    return out


def gelu_(b, out, x, t1, t2):
    b.tt(t1, x, x, ALU.mult)
    b.ts(t1, t1, 0.044715, 1.0, op0=ALU.mult, op1=ALU.add)
    b.tt(t1, t1, x, ALU.mult)
    b.act(t2, t1, AF.Sigmoid, scale=1.5957691216057308)
    b.tt(out, x, t2, ALU.mult)


def to_tokmajor(g, src, PW, n, dst_fn, nm="tk"):
    b = g.b
    stg = [b.sb("%s_stg%d" % (nm, i), [128, PW]) for i in range(2)]
    for t in range(n // 128):
        pp = g.pb[4 + t % 2]
        b.tr(pp[:, 0:PW], src[0:PW, t * 128:(t + 1) * 128], g.ident[0:PW, 0:PW])
        s_ = stg[t % 2]
        b.copy(s_, pp[:, 0:PW], e=("act" if t % 2 else "dve"))
        dst_fn(t, s_)


def stage_lru(g, l):
    b, c = g.b, g.c
    W, LT, LC, L = c.W, c.LT, c.LC, c.L
    PW = min(128, W)
    hp = PW // 64
    for ct in range(W // PW):
        b.push()
        ch0 = ct * PW
        pc = b.sb("lru_pc", [PW, 16])
        for j in range(4):
            colparam(g, pc, j, g.w["lru_conv_w"], (l * 4 + j) * W + ch0, PW)
        colparam(g, pc, 4, g.w["lru_conv_b"], l * W + ch0, PW)
        for d in range(2):
            colparam(g, pc, 5 + d, g.w["lru_br"], (l * 2 + d) * W + ch0, PW)
            colparam(g, pc, 7 + d, g.w["lru_bi"], (l * 2 + d) * W + ch0, PW)
            colparam(g, pc, 9 + d, g.w["lru_lam"], (l * 2 + d) * W + ch0, PW)
        b.act(pc[:, 11:13], pc[:, 9:11], AF.Exp, scale=-1.0)
        b.act(pc[:, 11:13], pc[:, 11:13], AF.Ln, bias=1.0)
        b.ts(pc[:, 11:13], pc[:, 11:13], -8.0, None, op0=ALU.mult)
        wbd = {}
        for d in range(2):
            for gi, gn in enumerate(("lru_wr", "lru_wi")):
                wt = b.sb("lru_w%d%d" % (d, gi), [PW, PW])
                b.memset(wt, 0.0)
                for h in range(hp):
                    b.dma(wt[h * 64:(h + 1) * 64, h * 64:(h + 1) * 64], g.w[gn][l, d, ct * hp + h])
                wbd[(d, gi)] = wt
        xz, gz, xc, r_, i_, a_, bb, h0, h1 = [b.sb("lru_t%d" % i, [PW, LT]) for i in range(9)]
        for bi in range(c.NB):
            b.dma(xz, g.zT[bi, c.off[0] + ch0: c.off[0] + ch0 + PW, :])
            b.dma(gz, g.zT[bi, c.off[1] + ch0: c.off[1] + ch0 + PW, :])
            b.ts(xc, xz, pc[:, 2:3], pc[:, 4:5], op0=ALU.mult, op1=ALU.add)
            for (s0, n) in ((0, LC), (LC, L)):
                for j in (0, 1, 3):
                    sh = j - 2
                    if sh < 0:
                        o_ = xc[:, s0 - sh: s0 + n]
                        i0 = xz[:, s0: s0 + n + sh]
                    else:
                        o_ = xc[:, s0: s0 + n - sh]
                        i0 = xz[:, s0 + sh: s0 + n]
                    b.stt(o_, i0, pc[:, j:j + 1], o_, ALU.mult, ALU.add)
            for d in range(2):
                for (s0, n) in nblocks(LT):
                    pp = g.pb[0]
                    b.mm(pp[0:PW, 0:n], wbd[(d, 0)], xc[:, s0:s0 + n])
                    b.act(r_[:, s0:s0 + n], pp[0:PW, 0:n], AF.Sigmoid, bias=pc[:, 5 + d:6 + d])
                    pp = g.pb[1]
                    b.mm(pp[0:PW, 0:n], wbd[(d, 1)], xc[:, s0:s0 + n])
                    b.act(i_[:, s0:s0 + n], pp[0:PW, 0:n], AF.Sigmoid, bias=pc[:, 7 + d:8 + d])
                b.act(a_, r_, AF.Exp, scale=pc[:, 11 + d:12 + d])
                b.tt(bb, a_, a_, ALU.mult)
                b.act(bb, bb, AF.Sqrt, scale=-1.0, bias=1.0)
                b.tt(bb, bb, i_, ALU.mult)
                b.tt(bb, bb, xc, ALU.mult)
                if d == 0:
                    b.scan(h0, a_, bb, 0.0)
                else:
                    b.scan(rev(h1[:, 0:LC]), rev(a_[:, 0:LC]), rev(bb[:, 0:LC]), 0.0)
                    b.scan(rev(h1[:, LC:LT]), rev(a_[:, LC:LT]), rev(bb[:, LC:LT]), h1[:, 0:1])
            b.tt(h0, h0, h1, ALU.add)
            gelu_(b, r_, gz, i_, a_)
            b.tt(h0, h0, r_, ALU.mult)
            to_tokmajor(g, h0, PW, LT, lambda t, s_: b.dma(g.ymix[bi, t * 128:(t + 1) * 128, ch0:ch0 + PW], s_), nm="lru")
        b.pop()


def conv_seg(b, out, x, pc, taps, pad_left, segs, bias_col):
    ctr = pad_left
    b.ts(out, x, pc[:, ctr:ctr + 1], bias_col, op0=ALU.mult, op1=ALU.add)
    for (s0, n) in segs:
        for j in range(taps):
            sh = j - pad_left
            if sh == 0:
                continue
            if sh < 0:
                o_ = out[:, s0 - sh: s0 + n]
                i0 = x[:, s0: s0 + n + sh]
            else:
                o_ = out[:, s0: s0 + n - sh]
                i0 = x[:, s0 + sh: s0 + n]
            b.stt(o_, i0, pc[:, j:j + 1], o_, ALU.mult, ALU.add)


def stage_mlstm(g, l):
    b, c = g.b, g.c
    W, H, LT, LC, L, GW = c.W, c.H, c.LT, c.LC, c.L, c.GW
    R = L // GW
    NT = LT // 128
    NTC = LC // 128
    b.push()
    io = b.sb("ml_io", [128, 128])
    b.k.op("pool", lambda E: E.iota(io, pattern=[[1, 128]], base=0, channel_multiplier=-1, allow_small_or_imprecise_dtypes=True), [], [key(io)])
    tri = [b.sb("ml_tri%d" % d, [128, 128]) for d in range(2)]
    b.ts(tri[0], io, 0.0, None, op0=ALU.is_ge)
    b.ts(tri[1], io, 0.0, None, op0=ALU.is_le)
    ones = b.sb("ml_ones", [H, LT])
    zeros = b.sb("ml_zeros", [H, LT])
    b.memset(ones, 1.0)
    b.memset(zeros, 0.0)
    gb = b.sb("ml_gb", [H, 8])
    for d in range(2):
        for f in range(2):
            colparam(g, gb, d * 2 + f, g.w["mlstm_gate_b"], ((l * 2 + d) * 2 + f) * H, H)
    b.ts(gb[:, 4:8], gb[:, 0:4], -1.0, None, op0=ALU.mult)
    pcq = [b.sb("ml_pc%d" % i, [64, 8]) for i in range(2 * H)]
    for qk in range(2):
        for h in range(H):
            t = pcq[qk * H + h]
            ch = qk * W + h * 64
            for j in range(4):
                colparam(g, t, j, g.w["mlstm_conv_w"], (l * 4 + j) * 2 * W + ch, 64)
            colparam(g, t, 4, g.w["mlstm_conv_b"], l * 2 * W + ch, 64)
    qT1 = b.sb("ml_q", [64, LT], BF16)
    kT1 = b.sb("ml_k", [64, LT], BF16)
    qT = [qT1] * H
    kT = [kT1] * H
    raw = b.sb("ml_raw", [64, LT])
    raw2 = b.sb("ml_raw2", [64, LT])
    igt = b.sb("ml_ig", [H, LT])
    fgt = b.sb("ml_fg", [H, LT])
    tmpg = b.sb("ml_tmpg", [H, LT])
    Lc = b.sb("ml_Lc", [H, LT])
    u_ = b.sb("ml_u", [H, LT])
    P_ = b.sb("ml_P", [H, LT])
    WB = [b.sb("ml_WB%d" % d, [128, LT]) for d in range(2)]
    ucol = b.sb("ml_ucol", [128, 2, NT])
    vaugb = b.sb("ml_vaugb", [128, NT, 65], BF16)
    emc = b.sb("ml_emc", [128, 2 * H, NT])
    Ex = [b.sb("ml_Ex%d" % i, [128, 512]) for i in range(2)]
    Sm = [b.sb("ml_Sm%d" % i, [128, 512], BF16) for i in range(2)]
    vaug = b.sb("ml_vaug", [128, NT, 65])
    osg = b.sb("ml_osg", [128, 64])
    hacc = b.sb("ml_hacc", [128, 4, 64])
    nd = b.sb("ml_nd", [128, 65])
    dm = b.sb("ml_dm", [128, 1])
    segs = ((0, LC), (LC, L))

    def cm(t_out, t_in, rows):
        b.copy(t_out[0:rows, 0:LC], t_in[0:rows, 0:LC], e="pool")
        b.copy(fview(t_out[0:rows, :], [[R, GW], [1, R]], off=LC), fview(t_in[0:rows, :], [[1, GW], [GW, R]], off=LC))

    def tokrows(dr, bi, t, c0, n):
        rs = dr.ap[1][0]
        base = dr[bi].offset
        if t < NTC:
            return mkap(dr, base + (t * 128) * rs + c0, [[rs, 128], [1, n]])
        n0 = (t - NTC) * 128
        w0 = n0 // R
        nw = 128 // R
        return mkap(dr, base + (LC + w0) * rs + c0, [[rs, nw], [GW * rs, R], [1, n]])

    for bi in range(c.NB):
        for d in range(2):
            r0 = c.off[12] + d * 2 * H
            b.dma(tmpg, g.zT[bi, r0:r0 + H, :])
            cm(igt, tmpg, H)
            b.dma(tmpg, g.zT[bi, r0 + H:r0 + 2 * H, :])
            cm(fgt, tmpg, H)
            b.ts(igt, igt, gb[:, d * 2:d * 2 + 1], None, op0=ALU.add)
            b.act(fgt, fgt, AF.Exp, scale=-1.0, bias=gb[:, 4 + d * 2 + 1: 4 + d * 2 + 2])
            b.act(fgt, fgt, AF.Ln, bias=1.0)
            if d == 0:
                b.scan(Lc, ones, fgt, 0.0)
            else:
                b.scan(rev(Lc[:, 0:LC]), rev(ones[:, 0:LC]), rev(fgt[:, 0:LC]), 0.0)
                b.scan(rev(Lc[:, LC:LT]), rev(ones[:, LC:LT]), rev(fgt[:, LC:LT]), Lc[:, 0:1])
            b.tt(u_, igt, Lc, ALU.add)
            if d == 0:
                b.scan(P_, u_, zeros, 0.0, op0=ALU.max, op1=ALU.max)
            else:
                b.scan(rev(P_[:, 0:LC]), rev(u_[:, 0:LC]), rev(zeros[:, 0:LC]), 0.0, op0=ALU.max, op1=ALU.max)
                b.scan(rev(P_[:, LC:LT]), rev(u_[:, LC:LT]), rev(zeros[:, LC:LT]), P_[:, 0:1], op0=ALU.max, op1=ALU.max)
            b.tt(tmpg, Lc, P_, ALU.subtract)
            b.act(tmpg, tmpg, AF.Exp)
            b.ts(P_, P_, -1.0, None, op0=ALU.mult)
            b.dma(g.mlsc2[0, d], u_)
            b.dma(g.mlsc2[1, d], P_)
            b.dma(g.mlsc[d], tmpg)
            for h in range(H):
                b.dma(emc[:, d * H + h, :], mkap(g.mlsc, (d * H + h) * LT, [[1, 128], [128, NT]]), allow_slow_non_contiguous=True)
        for h in range(H):
            for qk in range(2):
                row0 = c.off[8 + qk] + h * 64
                b.dma(raw, g.zT[bi, row0:row0 + 64, :])
                cm(raw2, raw, 64)
                dst = (qT if qk == 0 else kT)[h]
                pc = pcq[qk * H + h]
                conv_seg(b, raw, raw2, pc, 4, 2, segs, pc[:, 4:5])
                b.act(raw, raw, AF.Silu)
                b.ts(dst, raw, (0.125 if qk == 0 else 1.0), None, op0=ALU.mult)
            for d in range(2):
                b.dma(WB[d], mkap(g.mlsc2, ((1 * 2 + d) * H + h) * LT, [[0, 128], [1, LT]]))
                b.dma(ucol[:, d, :], mkap(g.mlsc2, ((0 * 2 + d) * H + h) * LT, [[1, 128], [128, NT]]), allow_slow_non_contiguous=True)
            b.memset(vaug, 1.0)
            for t in range(NT):
                b.dma(vaug[:, t, 0:64], tokrows(g.zt, bi, t, c.off[10] + h * 64, 64))
            b.copy(vaugb, vaug)
            def Jlist(I, d):
                if d == 0:
                    return list(range(0, I + 1))
                if I < NTC:
                    return list(range(I, NTC))
                return list(range(0, NTC)) + list(range(I, NT))
            QBS = 4
            blocks = [list(range(i0, min(i0 + QBS, NTC))) for i0 in range(0, NTC, QBS)] + \
                     [list(range(i0, min(i0 + QBS, NT))) for i0 in range(NTC, NT, QBS)]
            for QB in blocks:
                nq = len(QB)
                c0q, c1q = QB[0] * 128, (QB[-1] + 1) * 128
                for d in range(2):
                    jl = {I: Jlist(I, d) for I in QB}
                    Jall = sorted(set(j for I in QB for j in jl[I]))
                    for ji, J in enumerate(Jall):
                        ps_s = g.pb[ji % 2]
                        b.mm(ps_s[:, 0:nq * 128], kT[h][:, J * 128:(J + 1) * 128], qT[h][:, c0q:c1q])
                        ex = Ex[ji % 2]
                        sm = Sm[ji % 2]
                        b.act(ex[:, 0:nq * 128], WB[d][:, c0q:c1q], AF.Exp, bias=ucol[:, d, J:J + 1])
                        if J in QB:
                            qi = QB.index(J)
                            b.tt(ex[:, qi * 128:(qi + 1) * 128], ex[:, qi * 128:(qi + 1) * 128], tri[d], ALU.mult, e="pool")
                        b.tt(sm[:, 0:nq * 128], ps_s[:, 0:nq * 128], ex[:, 0:nq * 128], ALU.mult)
                        for qi, I in enumerate(QB):
                            if J in jl[I]:
                                b.mm(g.pb[2 + qi][:, 0:65], sm[:, qi * 128:(qi + 1) * 128], vaugb[:, J, :], start=(J == jl[I][0]), stop=(J == jl[I][-1]))
                    for qi, I in enumerate(QB):
                        b.copy(nd, g.pb[2 + qi][:, 0:65], e="act")
                        b.stt(dm, nd[:, 64:65], -1.0, nd[:, 64:65], ALU.mult, ALU.max)
                        b.ts(dm, dm, emc[:, d * H + h, I:I + 1], None, op0=ALU.max)
                        b.recip(dm, dm)
                        if d == 0:
                            b.ts(hacc[:, qi, :], nd[:, 0:64], dm, None, op0=ALU.mult)
                        else:
                            b.stt(hacc[:, qi, :], nd[:, 0:64], dm, hacc[:, qi, :], ALU.mult, ALU.add)
                for qi, I in enumerate(QB):
                    b.dma(osg, tokrows(g.zt, bi, I, c.off[11] + h * 64, 64))
                    b.act(osg, osg, AF.Sigmoid)
                    b.tt(osg, osg, hacc[:, qi, :], ALU.mult)
                    b.dma(tokrows(g.ymix, bi, I, 2 * W + h * 64, 64), osg)
    b.pop()


def hy_consts(c):
    import ml_dtypes
    out = {}
    for nm, Lu in (("c", c.LC), ("l", c.L)):
        n = np.arange(Lu, dtype=np.float64)
        f = np.arange(Lu, dtype=np.float64) + 0.5
        ang = np.pi * np.outer(n, f) / Lu
        NT_ = Lu // 128

        def blk4(T):
            return np.ascontiguousarray(T.reshape(NT_, 128, NT_, 128).transpose(2, 1, 0, 3)).reshape(NT_, 128, NT_ * 128).astype(ml_dtypes.bfloat16)
        out["hyCcf_" + nm] = blk4(np.cos(ang))
        out["hyCsf_" + nm] = blk4(np.sin(ang))
        out["hyCci_" + nm] = blk4(np.cos(ang).T)
        out["hyCsi_" + nm] = blk4(np.sin(ang).T)
        pos = np.arange(Lu, dtype=np.float32)
        t = pos / np.float32(Lu - 1)
        freqs = np.linspace(1e-4, 15, 16, dtype=np.float32)
        a2 = (np.float32(2 * np.pi / Lu) * pos[:, None] * freqs[None, :]).astype(np.float32)
        z = np.concatenate([t[:, None], np.cos(a2), -np.sin(a2)], -1).astype(np.float32)
        out["hyzfT_" + nm] = np.ascontiguousarray(z.T)
        out["hytcol_" + nm] = np.ascontiguousarray((-t).reshape(Lu // 128, 128).T)
    out["hydelta"] = np.abs(np.linspace(np.log(1e-2) / 1.5, np.log(1e-2) / 0.3, c.W, dtype=np.float32)).reshape(1, c.W)
    return out


def hy_decl(g):
    b, c = g.b, g.c
    g.hyc = {}
    for nm, Lu in (("c", c.LC), ("l", c.L)):
        for k_ in ("Ccf", "Csf", "Cci", "Csi"):
            g.hyc[k_ + nm] = b.dram("hy%s_%s" % (k_, nm), [Lu // 128, 128, Lu], BF16, kind="ExternalInput")
        g.hyc["zfT" + nm] = b.dram("hyzfT_" + nm, [33, Lu], kind="ExternalInput")
        g.hyc["tcol" + nm] = b.dram("hytcol_" + nm, [128, Lu // 128], kind="ExternalInput")
    g.hyc["delta"] = b.dram("hydelta", [1, c.W], kind="ExternalInput")
    g.hyu = b.dram("hyu", [c.LT, c.NB, 3 * c.W])
    g.hyz1 = b.dram("hyz1", [c.L, c.NB, c.W])


def stage_hyena(g, l):
    b, c = g.b, g.c
    W, LT, LC, L, NB = c.W, c.LT, c.LC, c.L, c.NB
    NBW = NB * W
    W3 = 3 * W
    PI = float(np.pi)
    b.push()
    cw = [b.sb("hy_cw%d" % j, [128, W3]) for j in range(3)]
    for j in range(3):
        b.dma(cw[j], mkap(g.w["hy_conv_w"], (l * 3 + j) * W3, [[0, 128], [1, W3]]))
    cb = b.sb("hy_cb", [128, W3])
    b.dma(cb, mkap(g.w["hy_conv_b"], l * W3, [[0, 128], [1, W3]]))
    cur = [b.sb("hy_cur%d" % i, [128, W3]) for i in range(2)]
    prv = [b.sb("hy_prv%d" % i, [128, W3]) for i in range(2)]
    nxt = [b.sb("hy_nxt%d" % i, [128, W3]) for i in range(2)]
    acc = [b.sb("hy_acc%d" % i, [128, W3]) for i in range(2)]
    it = 0
    for bi in range(NB):
        for (s0, n) in ((0, LC), (LC, L)):
            for t in range(n // 128):
                r0 = s0 + t * 128
                cu, pr, nx, ac = cur[it % 2], prv[it % 2], nxt[it % 2], acc[it % 2]
                it += 1
                c0 = c.off[13]
                b.dma(cu, g.zt[bi, r0:r0 + 128, c0:c0 + W3])
                if t == 0:
                    b.memset(pr, 0.0)
                    b.dma(pr[1:128, :], g.zt[bi, r0:r0 + 127, c0:c0 + W3])
                else:
                    b.dma(pr, g.zt[bi, r0 - 1:r0 + 127, c0:c0 + W3])
                if t == n // 128 - 1:
                    b.memset(nx, 0.0, e="pool")
                    b.dma(nx[0:127, :], g.zt[bi, r0 + 1:r0 + 128, c0:c0 + W3])
                else:
                    b.dma(nx, g.zt[bi, r0 + 1:r0 + 129, c0:c0 + W3])
                b.tt(ac, cu, cw[1], ALU.mult)
                b.tt(ac, ac, cb, ALU.add)
                b.tt(pr, pr, cw[0], ALU.mult, e="pool")
                b.tt(nx, nx, cw[2], ALU.mult, e="pool")
                b.tt(ac, ac, pr, ALU.add)
                b.tt(ac, ac, nx, ALU.add)
                b.dma(g.hyu[r0:r0 + 128, bi, :], ac)
    b.pop()
    for nm, s0, Lu in (("c", 0, LC), ("l", LC, L)):
        if nm == "c" and l == c.depth - 1 and not getattr(g, "force_ctx", False):
            continue
        NT = Lu // 128
        b.push()
        Ccf, Csf, Cci, Csi = [g.hyc[k_ + nm] for k_ in ("Ccf", "Csf", "Cci", "Csi")]
        zf = b.sb("hy_zf", [33, Lu])
        b.dma(zf, g.hyc["zfT" + nm])
        w1 = b.sb("hy_w1", [33, 64]); b.dma(w1, g.w["hy_w1"][l])
        w2 = b.sb("hy_w2", [64, 64]); b.dma(w2, g.w["hy_w2"][l])
        w3 = b.sb("hy_w3", [64, 4 * W]); b.dma(w3, g.w["hy_w3"][l])
        pcol = b.sb("hy_pcol", [64, 4])
        colparam(g, pcol, 0, g.w["hy_b1"], l * 64, 64)
        colparam(g, pcol, 1, g.w["hy_b2"], l * 64, 64)
        colparam(g, pcol, 2, g.w["hy_freq"], (l * 2 + 0) * 64, 64)
        colparam(g, pcol, 3, g.w["hy_freq"], (l * 2 + 1) * 64, 64)
        h1 = b.sb("hy_h1", [64, Lu])
        h2 = b.sb("hy_h2", [64, Lu])
        hcnt = b.sb("hy_hcnt", [64, Lu])
        for (wt, src, dst, bcol, fcol, K) in ((w1, zf, h1, 0, 2, 33), (w2, h1, h2, 1, 3, 64)):
            for (n0, n) in nblocks(Lu):
                pp = g.pb[0]
                b.mm(pp[0:64, 0:n], wt[0:K, :], src[0:K, n0:n0 + n])
                b.ts(dst[:, n0:n0 + n], pp[0:64, 0:n], pcol[:, bcol:bcol + 1], pcol[:, fcol:fcol + 1], op0=ALU.add, op1=ALU.mult)
            b.ts(dst, dst, 11.0 * PI, None, op0=ALU.add)
            b.memset(hcnt, 0.0)
            for m_ in range(1, 11):
                b.stt(hcnt, dst, 2.0 * PI * m_, hcnt, ALU.is_ge, ALU.add)
            b.stt(dst, hcnt, -2.0 * PI, dst, ALU.mult, ALU.add)
            b.act(dst, dst, AF.Sin, bias=g.negpi[0:64, :])
        AB = b.sb("hy_AB", [128, NT, 2, 2 * W], BF16)
        tcol = b.sb("hy_tcol", [128, NT]); b.dma(tcol, g.hyc["tcol" + nm])
        drow = b.sb("hy_drow", [128, W]); b.dma(drow, mkap(g.hyc["delta"], 0, [[0, 128], [1, W]]))
        ones = b.sb("hy_ones", [128, 128]); b.memset(ones, 1.0)
        dec = b.sb("hy_dec", [128, W])
        tp = b.sb("hy_tp", [128, 4 * W])
        ab = b.sb("hy_abs", [128, 4 * W])
        psum_abs = [g.pb[4], g.pb[5]]
        HW = 2 * W
        for t in range(NT):
            for hf in range(2):
                pp = g.pb[hf]
                b.mm(pp[:, 0:HW], h2[:, t * 128:(t + 1) * 128], w3[:, hf * HW:(hf + 1) * HW])
            b.act(dec, drow, AF.Exp, scale=tcol[:, t:t + 1])
            for hf in range(2):
                b.tt(tp[:, hf * HW:(hf + 1) * HW].rearrange("p (d c) -> p d c", d=2), g.pb[hf][:, 0:HW].rearrange("p (d c) -> p d c", d=2),
                     fview(dec, [[0, 2], [1, W]]), ALU.mult)
            if t == 0:
                for o in range(2):
                    b.memset(tp[0:1, o * HW + W:(o + 1) * HW], 0.0)
            b.stt(ab, tp, -1.0, tp, ALU.mult, ALU.max)
            for o in range(2):
                b.mm(psum_abs[o][:, 0:HW], ones, ab[:, o * HW:(o + 1) * HW], start=(t == 0), stop=(t == NT - 1))
                fw = tp[:, o * HW: o * HW + W]
                bw = tp[:, o * HW + W: (o + 1) * HW]
                b.tt(AB[:, t, 0, o * W:(o + 1) * W], fw, bw, ALU.add)
                b.tt(AB[:, t, 1, o * W:(o + 1) * W], fw, bw, ALU.subtract)
        rn = b.sb("hy_rn", [128, 2 * W])
        for o in range(2):
            b.copy(rn[:, o * W:(o + 1) * W], psum_abs[o][:, 0:W], e="act")
            b.tt(rn[:, o * W:(o + 1) * W], rn[:, o * W:(o + 1) * W], psum_abs[o][:, W:HW], ALU.add)
        b.ts(rn, rn, EPS, None, op0=ALU.add)
        b.recip(rn, rn)
        G = b.sb("hy_G", [128, NT, 2, 2 * W], BF16)
        blk = [b.sb("hy_blk%d" % i, [128, NT, 128], BF16) for i in range(4)]
        nb_ = 0
        for ft in range(NT):
            bc, bs = blk[(nb_ * 2) % 4], blk[(nb_ * 2 + 1) % 4]
            nb_ += 1
            b.dma(bc, Ccf[ft].rearrange("p (t f) -> p t f", f=128))
            b.dma(bs, Csf[ft].rearrange("p (t f) -> p t f", f=128), q="pool")
            for nt in range(NT):
                b.mm(g.pb[0][:, 0:2 * W], bc[:, nt, :], AB[:, nt, 0, :], start=(nt == 0), stop=(nt == NT - 1))
            for nt in range(NT):
                b.mm(g.pb[1][:, 0:2 * W], bs[:, nt, :], AB[:, nt, 1, :], start=(nt == 0), stop=(nt == NT - 1))
            b.tt(G[:, ft, 0, :], g.pb[0][:, 0:2 * W], rn, ALU.mult)
            b.stt(G[:, ft, 1, :], g.pb[1][:, 0:2 * W], -1.0, rn, ALU.mult, ALU.mult)
        X = b.sb("hy_X", [128, NT, NBW], BF16)
        Pre = b.sb("hy_Pre", [128, NT, NBW], BF16)
        Pin = b.sb("hy_Pin", [128, NT, NBW], BF16)
        ut = [b.sb("hy_ut%d" % i, [128, NB, W3]) for i in range(2)]
        z1t = [b.sb("hy_z1t%d" % i, [128, NB, W]) for i in range(2)]
        t1 = b.sb("hy_t1", [128, NBW])
        t2 = b.sb("hy_t2", [128, NBW])
        brow = [b.sb("hy_brow%d" % o, [128, W]) for o in range(2)]
        for o in range(2):
            b.dma(brow[o], mkap(g.w["hy_bias"], (l * 2 + o) * W, [[0, 128], [1, W]]))
        for t in range(NT):
            u_ = ut[t % 2]
            b.dma(u_, g.hyu[s0 + t * 128: s0 + (t + 1) * 128])
            b.copy(X[:, t, :].rearrange("p (b c) -> p b c", b=NB), u_[:, :, 0:W])
        for o in range(2):
            for ft in range(NT):
                bc, bs = blk[(nb_ * 2) % 4], blk[(nb_ * 2 + 1) % 4]
                nb_ += 1
                b.dma(bc, Ccf[ft].rearrange("p (t f) -> p t f", f=128))
                b.dma(bs, Csf[ft].rearrange("p (t f) -> p t f", f=128), q="pool")
                ure, uim = g.pb[0], g.pb[1]
                for st_ in range(NT):
                    b.mm(ure[:, 0:NBW], bc[:, st_, :], X[:, st_, :], start=(st_ == 0), stop=(st_ == NT - 1))
                for st_ in range(NT):
                    b.mm(uim[:, 0:NBW], bs[:, st_, :], X[:, st_, :], start=(st_ == 0), stop=(st_ == NT - 1))
                gre = fview(G[:, ft, 0, o * W:(o + 1) * W], [[0, NB], [1, W]])
                gim = fview(G[:, ft, 1, o * W:(o + 1) * W], [[0, NB], [1, W]])
                v3 = lambda a: a.rearrange("p (b c) -> p b c", b=NB)
                b.tt(v3(t1), v3(ure[:, 0:NBW]), gre, ALU.mult)
                b.tt(v3(t2), v3(uim[:, 0:NBW]), gim, ALU.mult)
                b.tt(Pre[:, ft, :], t1, t2, ALU.add)
                b.tt(v3(t1), v3(uim[:, 0:NBW]), gre, ALU.mult)
                b.tt(v3(t2), v3(ure[:, 0:NBW]), gim, ALU.mult)
                b.tt(Pin[:, ft, :], t1, t2, ALU.subtract)
            for tt_ in range(NT):
                bc, bs = blk[(nb_ * 2) % 4], blk[(nb_ * 2 + 1) % 4]
                nb_ += 1
                b.dma(bc, Cci[tt_].rearrange("p (t f) -> p t f", f=128))
                b.dma(bs, Csi[tt_].rearrange("p (t f) -> p t f", f=128), q="pool")
                yp = g.pb[2 + tt_ % 2]
                for ft in range(NT):
                    b.mm(yp[:, 0:NBW], bc[:, ft, :], Pre[:, ft, :], start=(ft == 0), stop=False)
                for ft in range(NT):
                    b.mm(yp[:, 0:NBW], bs[:, ft, :], Pin[:, ft, :], start=False, stop=(ft == NT - 1))
                u_ = ut[tt_ % 2]
                b.dma(u_, g.hyu[s0 + tt_ * 128: s0 + (tt_ + 1) * 128])
                z_ = z1t[tt_ % 2]
                v3 = lambda a: a.rearrange("p (b c) -> p b c", b=NB)
                bro = fview(brow[o], [[0, NB], [1, W]])
                if o == 0:
                    b.tt(v3(t1), u_[:, :, 0:W], bro, ALU.mult)
                    b.stt(t1, yp[:, 0:NBW], 1.0 / Lu, t1, ALU.mult, ALU.add)
                    b.tt(z_, v3(t1), u_[:, :, W:2 * W], ALU.mult)
                    b.copy(X[:, tt_, :].rearrange("p (b c) -> p b c", b=NB), z_, e="pool")
                    b.dma(g.hyz1[tt_ * 128:(tt_ + 1) * 128], z_)
                else:
                    b.dma(z_, g.hyz1[tt_ * 128:(tt_ + 1) * 128])
                    b.tt(v3(t1), z_, bro, ALU.mult)
                    b.stt(t1, yp[:, 0:NBW], 1.0 / Lu, t1, ALU.mult, ALU.add)
                    b.tt(v3(t2), v3(t1), u_[:, :, 2 * W:3 * W], ALU.mult)
                    for bi in range(NB):
                        b.dma(g.ymix[bi, s0 + tt_ * 128: s0 + (tt_ + 1) * 128, 3 * W:4 * W], t2[:, bi * W:(bi + 1) * W])
        b.pop()


def stage_merge(g, l):
    b, c = g.b, g.c
    D, KD, W, LT, LC, L = c.D, c.KD, c.W, c.LT, c.LC, c.L
    need_ctx = (l < c.depth - 1) or getattr(g, "force_ctx", False)
    wob = b.sb("mg_wob", [128, KD, D], BF16)
    wst = [b.sb("mg_wst%d" % i, [128, D]) for i in range(2)]
    for kd in range(KD):
        b.dma(wst[kd % 2], g.w["w_out"][l, kd * 128:(kd + 1) * 128, :])
        b.copy(wob[:, kd, :], wst[kd % 2], e=("dve" if kd % 2 else "pool"))
    gg = b.sb("mg_gg", [128, D])
    b.dma(gg, mkap(g.w["grp_g"], l * D, [[0, 128], [1, D]]))
    gate = b.sb("mg_gate", [128, D])
    ym = [b.sb("mg_ym%d" % i, [128, D]) for i in range(2)]
    xt = [b.sb("mg_xt%d" % i, [128, D]) for i in range(2)]
    yb = [b.sb("mg_yb%d" % i, [128, D], BF16) for i in range(2)]
    yT = [b.sb("mg_yT%d" % i, [128, KD, 128], BF16) for i in range(2)]
    junk = b.sb("mg_junk", [128, W])
    ss = b.sb("mg_ss", [128, 4])
    it = 0
    for bi in range(c.NB):
        for seg in range(2):
            if seg == 0 and not need_ctx:
                continue
            r = 2 if seg == 0 else bi
            Lu = LC if seg == 0 else L
            s0 = 0 if seg == 0 else LC
            b.dma(gate, mkap(g.modv, (r * 6 + 2) * D, [[0, 128], [1, D]]))
            for t in range(Lu // 128):
                r0 = s0 + t * 128
                y_, x_, yb_, yT_ = ym[it % 2], xt[it % 2], yb[it % 2], yT[it % 2]
                it += 1
                b.dma(y_, g.ymix[bi, r0:r0 + 128, :])
                if l == 0:
                    b.dma(x_, (g.ctx[bi, t * 128:(t + 1) * 128, :] if seg == 0 else g.x[bi, t * 128:(t + 1) * 128, :]), q="pool")
                else:
                    b.dma(x_, g.xres[bi, r0:r0 + 128, :], q="pool")
                for q_ in range(4):
                    b.act(junk, y_[:, q_ * W:(q_ + 1) * W], AF.Square, accum_out=ss[:, q_:q_ + 1])
                b.ts(ss, ss, 1.0 / W, EPS, op0=ALU.mult, op1=ALU.add)
                b.act(ss, ss, AF.Sqrt)
                b.recip(ss, ss)
                for q_ in range(4):
                    b.stt(yb_[:, q_ * W:(q_ + 1) * W], y_[:, q_ * W:(q_ + 1) * W], ss[:, q_:q_ + 1], gg[:, q_ * W:(q_ + 1) * W], ALU.mult, ALU.mult)
                pt = g.pbb[it % 2]
                for kd in range(KD):
                    b.tr(pt[:, kd * 128:(kd + 1) * 128], yb_[:, kd * 128:(kd + 1) * 128], g.identb)
                b.copy(yT_, pt[:, 0:KD * 128].rearrange("p (k t) -> p k t", k=KD), e="act")
                for hf in range((D + 511) // 512):
                    n0 = hf * 512
                    n = min(512, D - n0)
                    pp = g.pb[hf % 2]
                    for kd in range(KD):
                        b.mm(pp[:, 0:n], yT_[:, kd, :], wob[:, kd, n0:n0 + n], start=(kd == 0), stop=(kd == KD - 1))
                    b.tt(y_[:, n0:n0 + n], pp[:, 0:n], gate[:, n0:n0 + n], ALU.mult)
                b.tt(x_, x_, y_, ALU.add, e="pool")
                b.dma(g.xres[bi, r0:r0 + 128, :], x_)


def stage_final(g):
    b, c = g.b, g.c
    D, LC, L = c.D, c.LC, c.L
    gg = b.sb("fn_g", [128, D])
    b.dma(gg, mkap(g.w["final_g"], 0, [[0, 128], [1, D]]))
    xt = [b.sb("fn_x%d" % i, [128, D]) for i in range(2)]
    junk = b.sb("fn_junk", [128, D])
    ss = b.sb("fn_ss", [128, 1])
    it = 0
    for bi in range(c.NB):
        for t in range(L // 128):
            x_ = xt[it % 2]
            it += 1
            b.dma(x_, g.xres[bi, LC + t * 128: LC + (t + 1) * 128, :])
            b.act(junk, x_, AF.Square, accum_out=ss)
            b.ts(ss, ss, 1.0 / D, EPS, op0=ALU.mult, op1=ALU.add)
            b.act(ss, ss, AF.Sqrt)
            b.recip(ss, ss)
            b.stt(x_, x_, ss, gg, ALU.mult, ALU.mult)
            b.dma(g.out[bi, t * 128:(t + 1) * 128, :], x_)


def rw_decl(g):
    b, c = g.b, g.c
    g.puv16 = b.dram("puv16", [c.NK, 128, c.KD * 128 + c.D], BF16)
    g.rws = b.dram("rws", [c.NB, c.LT, 5, 2, c.W])
    g.vsd = b.dram("vsd", [c.NB, 2, c.W, c.LT])
    g.ysd = b.dram("ysd", [c.NB, 2, c.W, c.LT])
    g.rbon = b.dram("rbon", [c.NB, c.LT, c.W])
    g.rg = b.dram("rg", [c.NB, c.LT, c.W])


def stage_rwkv(g, l):
    b, c = g.b, g.c
    W, H, LT, LC, L, NB = c.W, c.H, c.LT, c.LC, c.L, c.NB
    W3 = 3 * W
    PW = min(128, W)
    NPW = W // PW
    segs = ((0, LC), (LC, L))
    b.push()
    def brow(name, src, off, n):
        t = b.sb(name, [128, n])
        b.dma(t, mkap(src, off, [[0, 128], [1, n]]))
        return t
    mu0 = b.sb("rw_mu0", [128, W3]); mu1 = b.sb("rw_mu1", [128, W3])
    for i in range(3):
        b.dma(mu0[:, i * W:(i + 1) * W], mkap(g.w["rwkv_mu"], ((l * 3 + i) * 2 + 0) * W, [[0, 128], [1, W]]))
        b.dma(mu1[:, i * W:(i + 1) * W], mkap(g.w["rwkv_mu"], ((l * 3 + i) * 2 + 1) * W, [[0, 128], [1, W]]))
    w0r = [brow("rw_w0%d" % d, g.w["rwkv_w0"], (l * 2 + d) * W, W) for d in range(2)]
    a0r = [brow("rw_a0%d" % d, g.w["rwkv_a0"], (l * 2 + d) * W, W) for d in range(2)]
    kkr = brow("rw_kkr", g.w["rwkv_kk"], l * W, W)
    kar = brow("rw_kar", g.w["rwkv_ka"], l * W, W)
    rkr = brow("rw_rkr", g.w["rwkv_rk"], l * W, W)
    lng = brow("rw_lng", g.w["rwkv_ln_g"], l * W, W)
    lnb = brow("rw_lnb", g.w["rwkv_ln_b"], l * W, W)
    w2 = [b.sb("rw_w2%d" % d, [32, W]) for d in range(2)]
    a2 = [b.sb("rw_a2%d" % d, [32, W]) for d in range(2)]
    for d in range(2):
        b.dma(w2[d], g.w["rwkv_w2"][l, d]); b.dma(a2[d], g.w["rwkv_a2"][l, d])
    g2 = b.sb("rw_g2", [64, W]); b.dma(g2, g.w["rwkv_g2"][l])
    def prep_set(i):
        return (b.sb("rw_cur%d" % i, [128, W3]), b.sb("rw_prv%d" % i, [128, W3]), b.sb("rw_nxt%d" % i, [128, W3]),
                b.sb("rw_lz%d" % i, [128, 128]), b.sb("rw_lza%d" % i, [32, 128]), b.sb("rw_lzg%d" % i, [64, 128]),
                b.sb("rw_kk%d" % i, [128, W]), b.sb("rw_t1%d" % i, [128, W]), b.sb("rw_t2%d" % i, [128, W]),
                b.sb("rw_a%d" % i, [128, W]), b.sb("rw_dec%d" % i, [128, W]), b.sb("rw_bon%d" % i, [128, W]),
                b.sb("rw_sh%d" % i, [128, H]), b.sb("rw_vT%d" % i, [PW, 128]))
    psets = [prep_set(0), prep_set(1)]
    pit = 0
    for bi in range(NB):
        for (s0, n) in segs:
            for t in range(n // 128):
                cur, prv, nxt, lz, lza, lzg, kk, t1, t2, a_, dec, bon, sh, vT = psets[pit % 2]
                pit += 1
                r0 = s0 + t * 128
                c0 = c.off[2]
                b.dma(cur, g.zt[bi, r0:r0 + 128, c0:c0 + W3])
                b.memset(prv, 0.0); b.memset(nxt, 0.0, e="pool")
                if t == 0:
                    b.dma(prv[1:128, :], g.zt[bi, r0:r0 + 127, c0:c0 + W3])
                else:
                    b.dma(prv, g.zt[bi, r0 - 1:r0 + 127, c0:c0 + W3])
                if t == n // 128 - 1:
                    b.dma(nxt[0:127, :], g.zt[bi, r0 + 1:r0 + 128, c0:c0 + W3])
                else:
                    b.dma(nxt, g.zt[bi, r0 + 1:r0 + 129, c0:c0 + W3])
                b.tt(prv, prv, cur, ALU.subtract); b.tt(prv, prv, mu0, ALU.mult)
                b.tt(nxt, nxt, cur, ALU.subtract, e="pool"); b.tt(nxt, nxt, mu1, ALU.mult, e="pool")
                b.tt(cur, cur, prv, ALU.add); b.tt(cur, cur, nxt, ALU.add)
                r_, k_, v_ = cur[:, 0:W], cur[:, W:2 * W], cur[:, 2 * W:3 * W]
                b.dma(lz, g.zT[bi, c.off[5]:c.off[5] + 128, r0:r0 + 128])
                b.dma(lza, g.zT[bi, c.off[6]:c.off[6] + 32, r0:r0 + 128])
                b.dma(lzg, g.zT[bi, c.off[7]:c.off[7] + 64, r0:r0 + 128])
                b.act(lz[0:32, :], lz[0:32, :], AF.Tanh)
                b.act(lzg, lzg, AF.Sigmoid)
                b.mm(g.pb[2][:, 0:W], lzg, g2)
                b.copy(t1, g.pb[2][:, 0:W], e="act")
                b.dma(g.rg[bi, r0:r0 + 128, :], t1)
                b.tt(kk, k_, kkr, ALU.mult)
                b.tt(t1, kk, kk, ALU.mult)
                b.reduce(sh, t1.rearrange("p (h k) -> p h k", h=H))
                b.act(sh, sh, AF.Sqrt)
                b.ts(sh, sh, 1e-12, None, op0=ALU.max)
                b.recip(sh, sh)
                b.tt(kk.rearrange("p (h k) -> p h k", h=H), kk.rearrange("p (h k) -> p h k", h=H), fview(sh, [[1, H], [0, 64]]), ALU.mult)
                for pw in range(NPW):
                    b.tr(g.pb[3][0:PW, 0:128], v_[:, pw * PW:(pw + 1) * PW], g.ident)
                    b.copy(vT, g.pb[3][0:PW, 0:128], e="act")
                    b.dma(g.vsd[bi, 0, pw * PW:(pw + 1) * PW, r0:r0 + 128], vT)
                    b.copy(vT, rev(g.pb[3][0:PW, 0:128]), e="act")
                    tlo = s0 + n - 128 - (r0 - s0)
                    b.dma(g.vsd[bi, 1, pw * PW:(pw + 1) * PW, tlo:tlo + 128], vT)
                for d in range(2):
                    b.mm(g.pb[0][:, 0:W], lz[0:32, :], w2[d])
                    b.mm(g.pb[1][:, 0:W], lza, a2[d])
                    b.tt(dec, g.pb[0][:, 0:W], w0r[d], ALU.add)
                    b.act(dec, dec, AF.Exp, scale=-1.0)
                    b.act(dec, dec, AF.Ln, bias=1.0)
                    b.act(dec, dec, AF.Exp, scale=-1.0, bias=g.neghalf)
                    b.act(dec, dec, AF.Exp, scale=-1.0)
                    b.tt(a_, g.pb[1][:, 0:W], a0r[d], ALU.add)
                    b.act(a_, a_, AF.Sigmoid)
                    b.stt(t1, a_, -1.0, kar, ALU.add, ALU.mult)
                    b.stt(t1, t1, 1.0, k_, ALU.add, ALU.mult)
                    b.tt(t2, kk, a_, ALU.mult)
                    rs = 5 * 2 * W
                    base = g.rws[bi].offset
                    def dst(comp):
                        return mkap(g.rws, base + r0 * rs + (comp * 2 + d) * W, [[rs, 128], [1, W]])
                    b.dma(dst(0), dec); b.dma(dst(1), kk); b.dma(dst(2), t2); b.dma(dst(3), t1); b.dma(dst(4), r_)
                    b.tt(t2, t1, r_, ALU.mult)
                    b.tt(t2, t2, rkr, ALU.mult)
                    b.reduce(sh, t2.rearrange("p (h k) -> p h k", h=H))
                    if d == 0:
                        b.tt(bon.rearrange("p (h k) -> p h k", h=H), v_.rearrange("p (h k) -> p h k", h=H), fview(sh, [[1, H], [0, 64]]), ALU.mult)
                    else:
                        b.tt(t2.rearrange("p (h k) -> p h k", h=H), v_.rearrange("p (h k) -> p h k", h=H), fview(sh, [[1, H], [0, 64]]), ALU.mult)
                        b.tt(bon, bon, t2, ALU.add)
                b.dma(g.rbon[bi, r0:r0 + 128, :], bon)
    b.pop()
    b.push()
    NP = NB * 64
    CH = 2 * H
    FS = CH * 64
    TB = 4
    VT = 128
    Ss = [b.sb("rs_S%d" % i, [NP, FS]) for i in range(2)]
    b.memset(Ss[0], 0.0); b.memset(Ss[1], 0.0)
    q3s = [b.sb("rs_q3%d" % i, [NP, FS]) for i in range(2)]
    yqs = [b.sb("rs_yq%d" % i, [NP, FS]) for i in range(2)]
    prev_r = None
    Bc = [b.sb("rs_Bc%d" % i, [NP, TB, 5, FS]) for i in range(3)]
    Vt = [b.sb("rs_Vt%d" % i, [NP, CH, VT]) for i in range(2)]
    Yb = [b.sb("rs_Yb%d" % i, [NP, CH, VT]) for i in range(2)]
    q1 = b.sb("rs_q1", [NP, FS]); q2 = b.sb("rs_q2", [NP, FS]); q3 = b.sb("rs_q3", [NP, FS]); q4 = b.sb("rs_q4", [NP, FS])
    sa = b.sb("rs_sa", [NP, CH])
    v3 = lambda a: a.rearrange("p (c k) -> p c k", c=CH)
    for tau in range(LT):
        blk, ti = divmod(tau, TB)
        if ti == 0:
            bc = Bc[blk % 3]
            tq = tau + TB - 1
            tok1 = (LC - 1 - tq) if tq < LC else (LC + (L - 1 - (tq - LC)))
            for bi in range(NB):
                b.dma(bc[bi * 64:(bi + 1) * 64, :, :, 0:W], mkap(g.rws, g.rws[bi].offset + tau * 5 * FS, [[0, 64], [5 * FS, TB], [FS, 5], [1, W]]), q=("sp" if bi == 0 else "act"))
                b.dma(bc[bi * 64:(bi + 1) * 64, :, :, W:FS], mkap(g.rws, g.rws[bi].offset + tok1 * 5 * FS + W, [[0, 64], [5 * FS, TB], [FS, 5], [1, W]]), q=("sp" if bi == 0 else "act"))
        vb, vi = divmod(tau, VT)
        if vi == 0:
            vt = Vt[vb % 2]; yb = Yb[vb % 2]
            for bi in range(NB):
                for h in range(H):
                    for d in range(2):
                        b.dma(vt[bi * 64:(bi + 1) * 64, d * H + h, :], g.vsd[bi, d, h * 64:(h + 1) * 64, tau:tau + VT])
        def comp(j):
            return mkap(bc, bc.offset + ti * 5 * FS + j * FS, [list(bc.ap[0]), [(TB - 1 - 2 * ti) * 5 * FS + W, 2], [64, H], [1, 64]])
        w_b, kk_b, kka_b, kd_b, r_b = [comp(j) for j in range(5)]
        v4 = lambda a: a.rearrange("p (d h k) -> p d h k", d=2, h=H)
        Sp, Sn = Ss[(tau + 1) % 2], Ss[tau % 2]
        q3_ = q3s[tau % 2]
        b.k.op("pool", lambda E: E.tensor_tensor(out=v4(q3_), in0=kd_b, in1=mkap(vt, vt.offset + vi, [list(vt.ap[0]), [H * VT, 2], [VT, H], [0, 64]]), op=ALU.mult), [key(bc), key(vt)], [key(q3_)])
        if prev_r is not None:
            yq_ = yqs[tau % 2]
            b.tt(v4(yq_), v4(Sp), prev_r[0], ALU.mult, e="pool")
        b.tt(v4(q1), v4(Sp), kk_b, ALU.mult)
        b.reduce(sa, v3(q1))
        b.tt(v4(q4), kka_b, mkap(sa, sa.offset, [list(sa.ap[0]), [H, 2], [1, H], [0, 64]]), ALU.mult)
        b.tt(q4, q4, q3_, ALU.subtract)
        b.tt(v4(q2), v4(Sp), w_b, ALU.mult)
        b.tt(Sn, q2, q4, ALU.subtract)
        if prev_r is not None:
            b.reduce(prev_r[1], v3(yq_))
        prev_r = (r_b, mkap(yb, yb.offset + vi, [list(yb.ap[0]), [VT, CH]]), yb)
        if tau == LT - 1 or vi == VT - 1:
            yq_ = yqs[(tau + 1) % 2]
            b.tt(v4(yq_), v4(Sn), prev_r[0], ALU.mult, e="pool")
            b.reduce(prev_r[1], v3(yq_))
            prev_r = None
        if vi == VT - 1 or tau == LT - 1:
            t0 = tau - vi
            for bi in range(NB):
                for h in range(H):
                    for d in range(2):
                        b.dma(g.ysd[bi, d, h * 64:(h + 1) * 64, t0:t0 + VT], yb[bi * 64:(bi + 1) * 64, d * H + h, :])
    b.pop()
    b.push()
    lng = brow("rw_lng2", g.w["rwkv_ln_g"], l * W, W)
    lnb = brow("rw_lnb2", g.w["rwkv_ln_b"], l * W, W)
    y0 = b.sb("ro_y0", [PW, 128]); y1 = b.sb("ro_y1", [PW, 128])
    yt = b.sb("ro_yt", [128, W]); t1 = b.sb("ro_t1", [128, W]); t2 = b.sb("ro_t2", [128, W])
    mu = b.sb("ro_mu", [128, H]); var = b.sb("ro_var", [128, H])
    h3 = lambda a: a.rearrange("p (h k) -> p h k", h=H)
    for bi in range(NB):
        for (s0, n) in segs:
            if s0 == 0 and l == c.depth - 1 and not getattr(g, "force_ctx", False):
                continue
            for t in range(n // 128):
                r0 = s0 + t * 128
                tlo = s0 + n - 128 - (r0 - s0)
                for pw in range(NPW):
                    b.dma(y0, g.ysd[bi, 0, pw * PW:(pw + 1) * PW, r0:r0 + 128])
                    b.dma(y1, g.ysd[bi, 1, pw * PW:(pw + 1) * PW, tlo:tlo + 128])
                    b.tt(y0, y0, rev(y1), ALU.add)
                    b.tr(g.pb[0][:, 0:PW], y0, g.ident[0:PW, 0:PW])
                    b.copy(yt[:, pw * PW:(pw + 1) * PW], g.pb[0][:, 0:PW], e="act")
                b.reduce(mu, h3(yt))
                b.ts(mu, mu, 1.0 / 64, None, op0=ALU.mult)
                b.tt(h3(t1), h3(yt), fview(mu, [[1, H], [0, 64]]), ALU.subtract)
                b.tt(t2, t1, t1, ALU.mult)
                b.reduce(var, h3(t2))
                b.ts(var, var, 1.0 / 64, 64e-5, op0=ALU.mult, op1=ALU.add)
                b.act(var, var, AF.Sqrt)
                b.recip(var, var)
                b.tt(h3(t1), h3(t1), fview(var, [[1, H], [0, 64]]), ALU.mult)
                b.tt(t1, t1, lng, ALU.mult)
                b.tt(t1, t1, lnb, ALU.add)
                b.dma(t2, g.rbon[bi, r0:r0 + 128, :])
                b.tt(t1, t1, t2, ALU.add)
                b.dma(t2, g.rg[bi, r0:r0 + 128, :])
                b.tt(t1, t1, t2, ALU.mult)
                b.dma(g.ymix[bi, r0:r0 + 128, W:2 * W], t1)
    b.pop()


def stage_peer(g, l):
    b, c = g.b, g.c
    D, KD, PH, NK, LT, LC, NB = c.D, c.KD, c.PH, c.NK, c.LT, c.LC, c.NB
    NJ = PH * 2
    need_ctx = (l < c.depth - 1) or getattr(g, "force_ctx", False)
    NHF = max(1, D // 512)
    HW_ = D // NHF
    NTB = 2
    b.push()
    uT_r_pre = g.w["peer_uT"][l].rearrange("(kd p) e -> p kd e", p=128)
    keysT = b.sb("pe_keysT", [128, NJ, NK])
    b.dma(keysT, g.w["peer_keysT"][l].rearrange("h c q k -> q (h c) k"))
    a2 = b.sb("pe_a2", [128, D]); s2 = b.sb("pe_s2", [128, D]); g5 = b.sb("pe_g5", [128, D])
    iot = b.sb("pe_iot", [128, 128])
    b.k.op("pool", lambda E: E.iota(iot, pattern=[[1, 128]], base=0, channel_multiplier=0, allow_small_or_imprecise_dtypes=True), [], [key(iot)])
    thr16 = b.sb("pe_thr16", [128, 16])
    b.ts(thr16, iot[:, 0:16], 16.0, 16.0, op0=ALU.mult, op1=ALU.add)
    xk = [b.sb("pe_xk%d" % i, [128, D]) for i in range(NTB)]
    junk = b.sb("pe_junk", [128, D])
    h2 = junk
    ss = b.sb("pe_ss", [128, 1])
    h2T = b.sb("pe_h2T", [128, KD, NTB * 128])
    wqc = [b.sb("pe_wqc%d" % i, [128, KD, 128]) for i in range(2)]
    qTs = b.sb("pe_qTs", [128, NTB * 128])
    scs = [b.sb("pe_sc%d" % i, [128, NJ, NK]) for i in range(NTB)]
    scw = b.sb("pe_scw", [128, 256])
    sv = b.sb("pe_sv", [128, NJ, 16])
    si = b.sb("pe_si", [128, NJ, 16], U32)
    sif = b.sb("pe_sif", [128, NJ, 16])
    cand = b.sb("pe_cand", [128, PH, 256])
    tsv = b.sb("pe_ts", [128, PH, 16])
    pos = b.sb("pe_pos", [128, PH, 16], U32)
    posf = b.sb("pe_posf", [128, PH, 16])
    aq = b.sb("pe_aq", [128, PH, 16]); bq = b.sb("pe_bq", [128, PH, 16])
    oh = b.sb("pe_oh", [128, PH, 16, 16])
    ik = b.sb("pe_ik", [128, PH * 16]); jk = b.sb("pe_jk", [128, PH * 16]); val = b.sb("pe_val", [128, PH * 16])
    zz = b.sb("pe_zz", [128, PH])
    KK = PH * 16
    ikT = b.sb("pe_ikT", [KK, 128]); jkT = b.sb("pe_jkT", [KK, 128]); valT = b.sb("pe_valT", [KK, 128])
    A = b.sb("pe_A", [KK, 64, NK], BF16)
    Bv = b.sb("pe_Bv", [KK, 64, NK], BF16)
    Wsb = b.sb("pe_Wsb", [128, NTB, 128, NK], BF16)
    UW = KD * 128
    uvc = [b.sb("pe_uvc%d" % i, [128, UW + D], BF16) for i in range(3)]
    uc = [t[:, 0:UW].rearrange("p (k e) -> p k e", k=KD) for t in uvc]
    vc = [t[:, UW:UW + D] for t in uvc]
    sgs = [b.sb("pe_sg%d" % i, [128, NTB * 128], BF16) for i in range(2)]
    G = [b.sb("pe_G%d" % i, [128, NTB * 128], BF16) for i in range(2)]
    h2Tb = b.sb("pe_h2Tb", [128, KD, NTB * 128], BF16)
    for i in range(NK):
        uf = wqc[i % 2]
        b.dma(uf, uT_r_pre[:, :, i * 128:(i + 1) * 128])
        ub = uc[i % 3]
        b.copy(ub, uf, e=("dve" if i % 2 else "pool"))
        b.dma(g.puv16[i, :, 0:UW], uvc[i % 3][:, 0:UW], q="act")
        vf = xk[i % 2]
        b.dma(vf, g.w["peer_v"][l, i * 128:(i + 1) * 128, :])
        vb = vc[i % 3]
        b.copy(vb, vf, e="act")
        b.dma(g.puv16[i, :, UW:UW + D], vb, q="act")
    wq_r = g.w["peer_wq"][l].rearrange("(kd p) c -> p kd c", p=128)
    uT_r = g.w["peer_uT"][l].rearrange("(kd p) e -> p kd e", p=128)
    mk_ = getattr(g, "mark", lambda n: None)
    mk_("pe:conv_done")
    for bi in range(NB):
        segs = [(2, 0, LC), (bi, LC, c.L)] if need_ctx else [(bi, LC, c.L)]
        for (r, s0, n) in segs:
            b.dma(a2, mkap(g.modv, (r * 6 + 4) * D, [[0, 128], [1, D]]))
            b.dma(s2, mkap(g.modv, (r * 6 + 3) * D, [[0, 128], [1, D]]))
            b.dma(g5, mkap(g.modv, (r * 6 + 5) * D, [[0, 128], [1, D]]))
            tiles = list(range(n // 128))
            for b0 in range(0, len(tiles), NTB):
                blk = tiles[b0:b0 + NTB]
                ntok = len(blk) * 128
                for ti, t in enumerate(blk):
                    mk_("pe:tile_start")
                    r0 = s0 + t * 128
                    x_ = xk[ti]
                    b.dma(x_, g.xres[bi, r0:r0 + 128, :])
                    b.act(junk, x_, AF.Square, accum_out=ss)
                    b.ts(ss, ss, 1.0 / D, EPS, op0=ALU.mult, op1=ALU.add)
                    b.act(ss, ss, AF.Sqrt)
                    b.recip(ss, ss)
                    b.stt(junk, x_, ss, a2, ALU.mult, ALU.mult)
                    b.tt(h2, junk, s2, ALU.add)
                    for kd in range(KD):
                        pp = g.pb[kd % 2]
                        b.tr(pp[:, 0:128], h2[:, kd * 128:(kd + 1) * 128], g.ident)
                        b.copy(h2T[:, kd, ti * 128:(ti + 1) * 128], pp[:, 0:128], e=("act" if kd % 2 else "dve"))
                for j in range(NJ):
                    wq_ = wqc[j % 2]
                    b.dma(wq_, wq_r[:, :, j * 128:(j + 1) * 128], q=("sp" if j % 2 == 0 else "act"))
                    pq = g.pb[2]
                    for kd in range(KD):
                        b.mm(pq[:, 0:ntok], wq_[:, kd, :], h2T[:, kd, 0:ntok], start=(kd == 0), stop=(kd == KD - 1))
                    b.copy(qTs[:, 0:ntok], pq[:, 0:ntok], e="act")
                    for ti in range(len(blk)):
                        b.mm(g.pb[3][:, ti * NK:(ti + 1) * NK], qTs[:, ti * 128:(ti + 1) * 128], keysT[:, j, :])
                        b.copy(scs[ti][:, j, :], g.pb[3][:, ti * NK:(ti + 1) * NK])
                for ti, t in enumerate(blk):
                    sc = scs[ti]
                    mk_("pe:scores_done")
                    for j in range(NJ):
                        b.k.op("dve", lambda E: E.max(out=sv[:, j, 0:8], in_=sc[:, j, :]), [key(sc)], [key(sv)])
                        b.k.op("dve", lambda E: E.max_index(out=si[:, j, 0:8], in_max=sv[:, j, 0:8], in_values=sc[:, j, :]), [key(sc), key(sv)], [key(si)])
                        b.k.op("dve", lambda E: E.match_replace(out=scw[:, 0:NK], in_to_replace=sv[:, j, 0:8], in_values=sc[:, j, :], imm_value=-1e30), [key(sc), key(sv)], [key(scw)])
                        b.k.op("dve", lambda E: E.max(out=sv[:, j, 8:16], in_=scw[:, 0:NK]), [key(scw)], [key(sv)])
                        b.k.op("dve", lambda E: E.max_index(out=si[:, j, 8:16], in_max=sv[:, j, 8:16], in_values=scw[:, 0:NK]), [key(scw), key(sv)], [key(si)])
                    b.copy(sif, si)
                    svf = sv.rearrange("p j r -> p (j r)")
                    b.tt(cand.rearrange("p h (a b) -> p h a b", a=16), fview(svf, [[32, PH], [1, 16], [0, 16]]), fview(svf, [[32, PH], [0, 16], [1, 16]], off=16), ALU.add)
                    for h in range(PH):
                        b.k.op("dve", lambda E: E.max(out=tsv[:, h, 0:8], in_=cand[:, h, :]), [key(cand)], [key(tsv)])
                        b.k.op("dve", lambda E: E.max_index(out=pos[:, h, 0:8], in_max=tsv[:, h, 0:8], in_values=cand[:, h, :]), [key(cand), key(tsv)], [key(pos)])
                        b.k.op("dve", lambda E: E.match_replace(out=scw, in_to_replace=tsv[:, h, 0:8], in_values=cand[:, h, :], imm_value=-1e30), [key(cand), key(tsv)], [key(scw)])
                        b.k.op("dve", lambda E: E.max(out=tsv[:, h, 8:16], in_=scw), [key(scw)], [key(tsv)])
                        b.k.op("dve", lambda E: E.max_index(out=pos[:, h, 8:16], in_max=tsv[:, h, 8:16], in_values=scw), [key(scw), key(tsv)], [key(pos)])
                    b.copy(posf, pos)
                    posff = posf.rearrange("p h r -> p (h r)")
                    b.tt(oh, fview(posff, [[16, PH], [1, 16], [0, 16]]), fview(thr16, [[0, PH], [0, 16], [1, 16]]), ALU.is_ge)
                    b.reduce(aq.rearrange("p h r -> p (h r)"), oh.rearrange("p h k a -> p (h k) a"))
                    b.stt(bq, aq, -16.0, posf, ALU.mult, ALU.add)
                    siff = sif.rearrange("p j r -> p (j r)")
                    for (src, cidx, dst) in ((aq, 0, ik), (bq, 1, jk)):
                        srcf = src.rearrange("p h r -> p (h r)")
                        b.tt(oh, fview(srcf, [[16, PH], [1, 16], [0, 16]]), fview(iot, [[0, PH], [0, 16], [1, 16]]), ALU.is_equal)
                        b.tt(oh, oh, fview(siff, [[32, PH], [0, 16], [1, 16]], off=cidx * 16), ALU.mult)
                        b.reduce(dst, oh.rearrange("p h k a -> p (h k) a"))
                    tsf = tsv.rearrange("p h r -> p (h r)")
                    b.tt(val.rearrange("p (h r) -> p h r", h=PH), tsv, fview(tsf, [[16, PH], [0, 16]]), ALU.subtract)
                    b.act(val, val, AF.Exp)
                    b.reduce(zz, val.rearrange("p (h r) -> p h r", h=PH))
                    b.recip(zz, zz)
                    b.tt(val.rearrange("p (h r) -> p h r", h=PH), val.rearrange("p (h r) -> p h r", h=PH), fview(zz, [[1, PH], [0, 16]]), ALU.mult)
                    for (src, dstT) in ((ik, ikT), (jk, jkT), (val, valT)):
                        b.tr(g.pb[2][0:KK, 0:128], src, g.ident)
                        b.copy(dstT, g.pb[2][0:KK, 0:128], e="act")
                    mk_("pe:route_done")
                    for hh in range(2):
                        b.tt(A, fview(ikT, [[1, 64], [0, NK]], off=hh * 64), fview(iot[0:KK, :], [[0, 64], [1, NK]]), ALU.is_equal)
                        b.tt(Bv, fview(jkT, [[1, 64], [0, NK]], off=hh * 64), fview(iot[0:KK, :], [[0, 64], [1, NK]]), ALU.is_equal)
                        b.tt(Bv, Bv, fview(valT, [[1, 64], [0, NK]], off=hh * 64), ALU.mult, e="pool")
                        for tk in range(64):
                            tkk = hh * 64 + tk
                            pw = g.pb[4 + (tk // 4) % 2]
                            b.mm(pw[:, (tk % 4) * 128:(tk % 4 + 1) * 128], Bv[:, tk, :], A[:, tk, :])
                            if tk % 4 == 3:
                                b.copy(Wsb[:, ti, tkk - 3:tkk + 1, :], pw.rearrange("p (t i) -> p t i", t=4), e=("act" if (tk // 4) % 2 else "dve"))
                mk_("pe:wbuild_done")
                po = [[g.pb[ti * NHF + hf] for hf in range(NHF)] for ti in range(len(blk))]
                b.copy(h2Tb[:, :, 0:ntok], h2T[:, :, 0:ntok])
                NBUF = len(uc)

                def load_tabs(i):
                    b.dma(uvc[i % NBUF], g.puv16[i], q=("sp" if i % 2 == 0 else "act"))

                def act_mm(i):
                    pa = g.pb[4 + i % 2]
                    for kd in range(KD):
                        b.mm(pa[:, 0:ntok], uc[i % NBUF][:, kd, :], h2Tb[:, kd, 0:ntok], start=(kd == 0), stop=(kd == KD - 1))

                for i in range(min(NBUF - 1, NK)):
                    load_tabs(i)
                act_mm(0)
                for i in range(NK):
                    if i + NBUF - 1 < NK:
                        load_tabs(i + NBUF - 1)
                    if i + 1 < NK:
                        act_mm(i + 1)
                    pa = g.pb[4 + i % 2]
                    sg = sgs[i % 2]
                    G_ = G[i % 2]
                    b.act(sg[:, 0:ntok], pa[:, 0:ntok], AF.Gelu_apprx_tanh)
                    b.tt(G_[:, 0:ntok].rearrange("p (t k) -> p t k", k=128), sg[:, 0:ntok].rearrange("p (t k) -> p t k", k=128),
                         mkap(Wsb, Wsb.offset + i, [list(Wsb.ap[0]), [128 * NK, len(blk)], [NK, 128]]), ALU.mult)
                    v_ = vc[i % NBUF]
                    for ti in range(len(blk)):
                        for hf in range(NHF):
                            b.mm(po[ti][hf][:, 0:HW_], G_[:, ti * 128:(ti + 1) * 128], v_[:, hf * HW_:(hf + 1) * HW_], start=(i == 0), stop=(i == NK - 1))
                mk_("pe:chunks_done")
                for ti, t in enumerate(blk):
                    r0 = s0 + t * 128
                    for hf in range(NHF):
                        b.tt(junk[:, hf * HW_:(hf + 1) * HW_], po[ti][hf][:, 0:HW_], g5[:, hf * HW_:(hf + 1) * HW_], ALU.mult)
                    b.tt(xk[ti], xk[ti], junk, ALU.add)
                    b.dma(g.xres[bi, r0:r0 + 128, :], xk[ti])
    b.pop()


_CACHE = {}


def build_program(cfg):
    g = setup(cfg)
    b = g.b
    for l in range(cfg.depth):
        for s in (stage_mod, stage_in, stage_lru, stage_rwkv, stage_mlstm, stage_hyena, stage_merge):
            b.push()
            s(g, l)
            b.pop()
        stage_peer(g, l)
    b.push()
    stage_final(g)
    b.pop()
    b.k.finish()
    return g


def kernel(**inputs):
    cfg = Cfg()
    inp = {k: np.ascontiguousarray(np.asarray(v, dtype=np.float32)) for k, v in inputs.items()}
    if "g" not in _CACHE:
        _CACHE["g"] = build_program(cfg)
    g = _CACHE["g"]
    shared = {n: inp[n] for n in WNAMES}
    shared.update(host_layout(inp))
    shared.update(hy_consts(cfg))
    n_cores = 8
    in_maps = []
    for i in range(n_cores):
        rows = np.stack([inp["c"][2 * i], inp["c"][2 * i + 1], inp["c_ctx"]], 0)
        c3T = np.ascontiguousarray(rows.T.reshape(cfg.KD, 128, 3).transpose(1, 0, 2))
        m = dict(shared)
        m["x"] = np.ascontiguousarray(inp["x"][2 * i:2 * i + 2])
        m["ctx"] = np.ascontiguousarray(inp["ctx"][2 * i:2 * i + 2])
        m["c3T"] = c3T
        in_maps.append(m)
    res = run_bass_kernel_spmd(g.nc, in_maps, core_ids=list(range(n_cores)))
    return np.concatenate([np.asarray(r["out"], dtype=np.float32) for r in res.results], axis=0)
```
